# Optimizing a Trainium2 kernel written in Bass

```python
import jax, jax.numpy as jnp
from jax import lax
import numpy as np

D_MODEL = 1024
BATCH = 8
SEQ = 4096
DEPTH = 4

CHUNK = 64
Q_BLOCK = 128
HEAD_DIM = 64
A_HEADS = D_MODEL // HEAD_DIM
A_KV_HEADS = 4
IDX_HEADS = 8
IDX_DIM = 64
TOPK_MAX = 256
B_HEADS = D_MODEL // HEAD_DIM
D_FF = 4 * D_MODEL
ROPE_THETA = 10000.0
EPS = 1e-6
N_A = DEPTH // 2
N_B = DEPTH - N_A

A_Q = A_HEADS * HEAD_DIM
A_KV = A_KV_HEADS * HEAD_DIM
A_QI = IDX_HEADS * IDX_DIM
A_IN = A_Q + 2 * A_KV + A_QI + IDX_DIM + IDX_HEADS
A_SPLITS = [A_Q, A_Q + A_KV, A_Q + 2 * A_KV, A_Q + 2 * A_KV + A_QI, A_Q + 2 * A_KV + A_QI + IDX_DIM]
B_QK = B_HEADS * HEAD_DIM
B_KV_IN = 2 * B_QK + B_HEADS

kernel_name = "yoco_dsa_fox_hybrid"


def rms_norm(x, g):
    x32 = x.astype(jnp.float32)
    y = x32 * lax.rsqrt(jnp.mean(x32 * x32, axis=-1, keepdims=True) + EPS)
    return (y * g.astype(jnp.float32)).astype(x.dtype)


def apply_rope(x):
    s, d = x.shape[1], x.shape[-1]
    half = d // 2
    inv = 1.0 / (ROPE_THETA ** (jnp.arange(0, d, 2, dtype=jnp.float32) / d))
    ang = jnp.arange(s, dtype=jnp.float32)[:, None] * inv[None, :]
    c = jnp.cos(ang)[None, :, None, :]
    sn = jnp.sin(ang)[None, :, None, :]
    x1 = x[..., :half].astype(jnp.float32)
    x2 = x[..., half:].astype(jnp.float32)
    return jnp.concatenate([x1 * c - x2 * sn, x1 * sn + x2 * c], axis=-1).astype(x.dtype)


def to_blocks(a):
    b, s = a.shape[:2]
    return jnp.moveaxis(a.reshape(b, s // Q_BLOCK, Q_BLOCK, *a.shape[2:]), 1, 0)


def from_blocks(a):
    a = jnp.moveaxis(a, 0, 1)
    return a.reshape(a.shape[0], a.shape[1] * a.shape[2], *a.shape[3:])


def dsa_attention(xn, w_in, w_o):
    b, s, _ = xn.shape
    q, k, v, qi, ki, wi = jnp.split(xn @ w_in, A_SPLITS, axis=-1)
    q = apply_rope(q.reshape(b, s, A_HEADS, HEAD_DIM))
    k = apply_rope(k.reshape(b, s, A_KV_HEADS, HEAD_DIM))
    v = v.reshape(b, s, A_KV_HEADS, HEAD_DIM)
    qi = apply_rope(qi.reshape(b, s, IDX_HEADS, IDX_DIM))
    ki = apply_rope(ki[:, :, None, :])[:, :, 0]
    wi = wi * (IDX_HEADS ** -0.5 * IDX_DIM ** -0.5)
    n_sel = min(TOPK_MAX, s // 4)
    key_pos = jnp.arange(s)
    t_pos = key_pos.reshape(s // Q_BLOCK, Q_BLOCK)
    group = A_HEADS // A_KV_HEADS
    scale = HEAD_DIM ** -0.5

    def block(args):
        qb, qib, wib, tb = args
        limit = (tb // CHUNK + 1) * CHUNK
        admissible = key_pos[None, :] < limit[:, None]
        idx_logits = jnp.einsum('bqhd,bsd->bqhs', qib, ki).astype(jnp.float32)
        score = jnp.einsum('bqhs,bqh->bqs', jax.nn.relu(idx_logits), wib.astype(jnp.float32))
        score = jnp.where(admissible[None], score, -jnp.inf)
        _, sel = lax.top_k(score, n_sel)
        valid = sel < limit[None, :, None]
        k_sel = jax.vmap(lambda kk, ii: kk[ii])(k, sel)
        v_sel = jax.vmap(lambda vv, ii: vv[ii])(v, sel)
        qg = qb.reshape(b, Q_BLOCK, A_KV_HEADS, group, HEAD_DIM)
        logits = jnp.einsum('bqngd,bqknd->bqngk', qg, k_sel).astype(jnp.float32) * scale
        logits = jnp.where(valid[:, :, None, None, :], logits, -jnp.inf)
        p = jax.nn.softmax(logits, axis=-1).astype(v.dtype)
        o = jnp.einsum('bqngk,bqknd->bqngd', p, v_sel)
        return o.reshape(b, Q_BLOCK, A_Q)

    o = lax.map(block, (to_blocks(q), to_blocks(qi), to_blocks(wi), t_pos))
    return from_blocks(o) @ w_o


def shared_kv(h, g_kv, w_kv, b_f):
    b, s, _ = h.shape
    k, v, f_logit = jnp.split(rms_norm(h, g_kv) @ w_kv, [B_QK, 2 * B_QK], axis=-1)
    log_f = jax.nn.log_sigmoid((f_logit + b_f).astype(jnp.float32))
    cum = jnp.moveaxis(jnp.cumsum(log_f, axis=1), 2, 1)
    return (k.reshape(b, s, B_HEADS, HEAD_DIM), v.reshape(b, s, B_HEADS, HEAD_DIM), cum)


def fox_attention(xn, w_q, w_o, k, v, cum):
    b, s, _ = xn.shape
    q = (xn @ w_q).reshape(b, s, B_HEADS, HEAD_DIM)
    key_pos = jnp.arange(s)
    t_pos = key_pos.reshape(s // Q_BLOCK, Q_BLOCK)
    cum_q = to_blocks(jnp.moveaxis(cum, 1, 2))
    scale = HEAD_DIM ** -0.5

    def block(args):
        qb, cqb, tb = args
        logits = jnp.einsum('bqhd,bshd->bhqs', qb, k).astype(jnp.float32) * scale
        decay = jnp.moveaxis(cqb, 1, 2)[..., None] - cum[:, :, None, :]
        causal = key_pos[None, :] <= tb[:, None]
        logits = jnp.where(causal[None, None], logits + decay, -jnp.inf)
        p = jax.nn.softmax(logits, axis=-1).astype(v.dtype)
        o = jnp.einsum('bhqs,bshd->bqhd', p, v)
        return o.reshape(b, Q_BLOCK, B_QK)

    o = lax.map(block, (to_blocks(q), cum_q, t_pos))
    return from_blocks(o) @ w_o


def sq_relu_mlp(xn, w_up, w_down):
    return jnp.square(jax.nn.relu(xn @ w_up)) @ w_down


def setup_inputs(seed: int = 0) -> dict:
    key = jax.random.key(seed)
    ks = jax.random.split(key, 16)
    f32 = jnp.float32

    def w(k, shape, fan_in):
        return jax.random.normal(k, shape, f32) * (fan_in ** -0.5)

    def gain(k, shape):
        return 1.0 + 0.01 * jax.random.normal(k, shape, f32)

    return {
        "x": jax.random.normal(ks[0], (BATCH, SEQ, D_MODEL), f32),
        "g_attn_a": gain(ks[1], (N_A, D_MODEL)),
        "w_in_a": w(ks[2], (N_A, D_MODEL, A_IN), D_MODEL),
        "w_o_a": w(ks[3], (N_A, A_Q, D_MODEL), A_Q),
        "g_kv": gain(ks[4], (D_MODEL,)),
        "w_kv_b": w(ks[5], (D_MODEL, B_KV_IN), D_MODEL),
        "b_f": jax.random.uniform(ks[6], (B_HEADS,), f32, minval=1.0, maxval=5.0),
        "g_attn_b": gain(ks[7], (N_B, D_MODEL)),
        "w_q_b": w(ks[8], (N_B, D_MODEL, B_QK), D_MODEL),
        "w_o_b": w(ks[9], (N_B, B_QK, D_MODEL), B_QK),
        "g_mlp": gain(ks[10], (DEPTH, D_MODEL)),
        "w_up": w(ks[11], (DEPTH, D_MODEL, D_FF), D_MODEL),
        "w_down": w(ks[12], (DEPTH, D_FF, D_MODEL), D_FF),
        "g_final": gain(ks[13], (D_MODEL,)),
    }


def reference(x, g_attn_a, w_in_a, w_o_a, g_kv, w_kv_b, b_f, g_attn_b, w_q_b, w_o_b,
              g_mlp, w_up, w_down, g_final):
    h = x
    k_sh = v_sh = cum_sh = None
    for layer in range(DEPTH):
        if layer < N_A:
            h = h + dsa_attention(rms_norm(h, g_attn_a[layer]), w_in_a[layer], w_o_a[layer])
        else:
            if layer == N_A:
                k_sh, v_sh, cum_sh = shared_kv(h, g_kv, w_kv_b, b_f)
            j = layer - N_A
            h = h + fox_attention(rms_norm(h, g_attn_b[j]), w_q_b[j], w_o_b[j], k_sh, v_sh, cum_sh)
        h = h + sq_relu_mlp(rms_norm(h, g_mlp[layer]), w_up[layer], w_down[layer])
    return rms_norm(h, g_final)
```

```python
import contextlib
import numpy as np
import concourse.bass as bass
import concourse.mybir as mybir
from concourse.bass_utils import run_bass_kernel_spmd

F32 = mybir.dt.float32
BF16 = mybir.dt.bfloat16
AF = mybir.ActivationFunctionType
ALU = mybir.AluOpType
AX = mybir.AxisListType

D = 1024
DFF = 4096
A_IN = 2120
KV_IN = 2064
EPS = 1e-6
NB = 14
TOPK = 256
BIG = 1.0e30


class Res:
    __slots__ = ("w", "r")

    def __init__(self):
        self.w = {}
        self.r = {}


class Sch:
    def __init__(self, nc, ctx):
        self.nc = nc
        self.eng = {"pe": nc.tensor, "act": nc.scalar, "dve": nc.vector, "pool": nc.gpsimd, "sp": nc.sync}
        self.semobj = {}
        for k in self.eng:
            self.semobj[k] = ctx.enter_context(nc.semaphore("s_" + k))
        self.cnt = {k: 0 for k in self.eng}
        self.seen = {k: {} for k in self.eng}
        self.NR = 16
        self.dcount = {"sp": 0, "pool": 0}
        for q in ("sp", "pool"):
            for i in range(self.NR):
                self.semobj[(q, i)] = ctx.enter_context(nc.semaphore("d_%s%d" % (q, i)))

    def _deps(self, e, reads, writes):
        d = {}

        def add(k, v, same_ok):
            if k == e and e == "pe":
                return
            if d.get(k, 0) < v:
                d[k] = v

        for r in reads:
            for k, v in r.w.items():
                add(k, v, True)
        for w in writes:
            for k, v in w.w.items():
                add(k, v, False)
            for k, v in w.r.items():
                add(k, v, False)
        return d

    def _wait(self, e, deps):
        sn = self.seen[e]
        for k, v in deps.items():
            if sn.get(k, 0) < v:
                self.eng[e].wait_ge(self.semobj[k], v)
                sn[k] = v

    def _mark(self, tag, reads, writes):
        k, v = tag
        for r in reads:
            if r.r.get(k, 0) < v:
                r.r[k] = v
        for w in writes:
            if w.w.get(k, 0) < v:
                w.w[k] = v
            w.r = {}

    def op(self, e, fn, reads=(), writes=(), inc=True):
        self._wait(e, self._deps(e, reads, writes))
        ins = fn(self.eng[e])
        if inc:
            self.cnt[e] += 1
            ins.then_inc(self.semobj[e], 1)
            tag = (e, self.cnt[e])
        else:
            tag = (e, self.cnt[e] + 1)
        self._mark(tag, reads, writes)

    def dma(self, q, out, in_, reads=(), writes=(), slow=False):
        j = self.dcount[q]
        self.dcount[q] += 1
        k = (q, j % self.NR)
        val = 16 * (j // self.NR + 1)
        deps = self._deps(q, reads, writes)
        if val > 16:
            deps[k] = max(deps.get(k, 0), val - 16)
        self._wait(q, deps)
        if slow:
            self.eng[q].dma_start(out=out, in_=in_, allow_slow_non_contiguous=True).then_inc(self.semobj[k], 16)
        else:
            self.eng[q].dma_start(out=out, in_=in_).then_inc(self.semobj[k], 16)
        self._mark((k, val), reads, writes)

    def barrier(self):
        tot = {}
        for k in self.eng:
            if self.cnt[k] > 0:
                tot[k] = self.cnt[k]
        for q in ("sp", "pool"):
            n = self.dcount[q]
            for i in range(self.NR):
                uses = (n - i + self.NR - 1) // self.NR if n > i else 0
                if uses > 0:
                    tot[(q, i)] = 16 * uses
        for e in self.eng:
            self._wait(e, {k: v for k, v in tot.items() if k != e})


def build(S=4096, stop=None, debug=False):
    NT = S // 128
    NS = S // 512
    NQ = S // 256
    nc = bass.Bass("TRN2", target_bir_lowering=False)

    def din(name, shape, dt=F32):
        return nc.dram_tensor(name, list(shape), dt, kind="ExternalInput").ap()

    x = din("x", [S, D])
    g_attn_a = din("g_attn_a", [2, D])
    w_in_a = din("w_in_a", [2, D, A_IN])
    w_o_a = din("w_o_a", [2, D, D])
    g_kv = din("g_kv", [D])
    w_kv_b = din("w_kv_b", [D, KV_IN])
    b_f = din("b_f", [16])
    g_attn_b = din("g_attn_b", [2, D])
    w_q_b = din("w_q_b", [2, D, D])
    w_o_b = din("w_o_b", [2, D, D])
    g_mlp = din("g_mlp", [4, D])
    w_up = din("w_up", [4, D, DFF])
    w_down = din("w_down", [4, DFF, D])
    g_final = din("g_final", [D])
    c_ident = din("c_ident", [128, 128])
    c_ropeC = din("c_ropeC", [128, S])
    c_ropeS = din("c_ropeS", [128, S])
    c_mbA = din("c_mbA", [128, 2, 256])
    c_cbB = din("c_cbB", [128, 4, 512])
    c_pw = din("c_pw", [128, 2 * (NB + 1)])
    out = nc.dram_tensor("out", [S, D], F32, kind="ExternalOutput").ap()

    dk = dict(kind="ExternalOutput") if debug else {}
    hT = nc.dram_tensor("hT", [D, S], F32, **dk).ap()
    qA = nc.dram_tensor("qA", [D, S], BF16, **dk).ap()
    qiA = nc.dram_tensor("qiA", [512, S], BF16, **dk).ap()
    dbg = nc.dram_tensor("dbg", [128, 8192], F32, **dk).ap()
    kaug = nc.dram_tensor("kaug", [16, 70, S], BF16).ap()
    qaug = nc.dram_tensor("qaug", [16, 70, S], BF16).ap()
    vB = nc.dram_tensor("vB", [S, 16, 65], BF16).ap()
    oTs = nc.dram_tensor("oTs", [16, 64, S], BF16).ap()
    hT_v = hT.rearrange("(c p) t -> p c t", p=128)

    with contextlib.ExitStack() as gctx:
        sc = Sch(nc, gctx)

        uid = [0]

        def sb(ctx, name, shape, dt):
            uid[0] += 1
            return ctx.enter_context(nc.sbuf_tensor("%s_u%d" % (name, uid[0]), list(shape), dt))

        P = [gctx.enter_context(nc.psum_tensor("P%d" % i, [128, 512], F32)) for i in range(7)]
        PB = gctx.enter_context(nc.psum_tensor("PB", [128, 1024], BF16))
        RP = [Res() for _ in range(7)]
        RPB = Res()

        ident_f = sb(gctx, "ident_f", [128, 128], F32)
        ident_b = sb(gctx, "ident_b", [128, 128], BF16)
        ones_b = sb(gctx, "ones_b", [128, 128], BF16)
        ones_f = sb(gctx, "ones_f", [128, 64], F32)
        eps_t = sb(gctx, "eps_t", [128, 1], F32)
        one_t = sb(gctx, "one_t", [128, 1], F32)
        gall = sb(gctx, "gall", [128, 10, 8], F32)
        Rc = Res()
        Rg = Res()
        sc.dma("sp", ident_f[:, :], c_ident, writes=[Rc])
        sc.op("dve", lambda e: e.tensor_copy(out=ident_b[:, :], in_=ident_f[:, :]), reads=[Rc], writes=[Rc])
        sc.op("dve", lambda e: e.memset(ones_b[:, :], 1.0), writes=[Rc])
        sc.op("dve", lambda e: e.memset(ones_f[:, :], 1.0), writes=[Rc])
        sc.op("dve", lambda e: e.memset(eps_t[:, :], EPS), writes=[Rc])
        sc.op("dve", lambda e: e.memset(one_t[:, :], 1.0), writes=[Rc])
        glist = [g_attn_a[0], g_attn_a[1], g_kv, g_attn_b[0], g_attn_b[1],
                 g_mlp[0], g_mlp[1], g_mlp[2], g_mlp[3], g_final]
        for gi, gap in enumerate(glist):
            sc.dma("sp", gall[:, gi, :], gap.rearrange("(c p) -> p c", p=128), writes=[Rg], slow=True)
        G_A, G_KV, G_B, G_M, G_F = 0, 2, 3, 5, 9
        RhT = Res()

        def load_w(q_rows_ap, dst, kparts, res):
            v = q_rows_ap.rearrange("(k p) n -> p k n", p=128)
            for k in range(kparts):
                sc.dma("pool", dst[:, k, :], v[:, k, :], writes=[res])

        def norm_tile(ctx_bufs, ht, Rht, gi, xn, Rxn, TW, final=False):
            sq, Rsq, rstd, Rrstd, sstd, Rsstd, bank = ctx_bufs
            sc.op("act", lambda e: e.activation(out=sq[:, :, 0:TW], in_=ht[:, :, 0:TW], func=AF.Square),
                  reads=[Rht], writes=[Rsq])
            for c in range(8):
                sc.op("pe", lambda e, c=c: e.matmul(P[bank][:, 0:TW], lhsT=ones_b[:, :], rhs=sq[:, c, 0:TW],
                                                   start=(c == 0), stop=(c == 7)),
                      reads=[Rsq, Rc], writes=[RP[bank]], inc=(c == 7))
            sc.op("act", lambda e: e.activation(out=sstd[:, 0:TW], in_=P[bank][:, 0:TW], func=AF.Sqrt,
                                                bias=eps_t[:, :], scale=1.0 / D),
                  reads=[RP[bank], Rc], writes=[Rsstd])
            sc.op("dve", lambda e: e.reciprocal(out=rstd[:, 0:TW], in_=sstd[:, 0:TW]),
                  reads=[Rsstd], writes=[Rrstd])
            for c in range(8):
                sc.op("dve", lambda e, c=c: e.scalar_tensor_tensor(
                    out=xn[:, c, 0:TW], in0=ht[:, c, 0:TW], scalar=gall[:, gi, c:c + 1], in1=rstd[:, 0:TW],
                    op0=ALU.mult, op1=ALU.mult), reads=[Rht, Rrstd, Rg], writes=[Rxn])

        def norm_bufs(ctx, TW, bank):
            sq = sb(ctx, "n_sq", [128, 8, TW], BF16)
            rstd = sb(ctx, "n_rstd", [128, TW], F32)
            sstd = sb(ctx, "n_sstd", [128, TW], F32)
            return (sq, Res(), rstd, Res(), sstd, Res(), bank)

        def normalize_heads(Ops, ROps, W_, Osb, ROsb, rden, Rrden, dst_ap, Rdst, bcbank, use_act=False):
            if rden is None:
                rden, Rrden = Osb, ROsb
            sc.op("act", lambda e: e.copy(out=Osb[0:65, 0:W_], in_=Ops[0:65, 0:W_]), reads=[ROps], writes=[ROsb])
            if use_act:
                sc.op("act", lambda e: e.activation(out=rden[64:65, 0:W_], in_=Osb[64:65, 0:W_], func=AF.Ln),
                      reads=[ROsb], writes=[Rrden])
                sc.op("act", lambda e: e.activation(out=rden[64:65, 0:W_], in_=rden[64:65, 0:W_], func=AF.Exp,
                                                    scale=-1.0), reads=[Rrden], writes=[Rrden])
            else:
                sc.op("dve", lambda e: e.reciprocal(out=rden[64:65, 0:W_], in_=Osb[64:65, 0:W_]),
                      reads=[ROsb], writes=[Rrden])
            sc.op("pe", lambda e: e.matmul(P[bcbank][0:64, 0:W_], lhsT=ones_f[64:65, 0:64], rhs=rden[64:65, 0:W_],
                                           start=True, stop=True), reads=[Rrden, Rc], writes=[RP[bcbank]])
            sc.op("dve", lambda e: e.tensor_tensor(out=dst_ap, in0=Osb[0:64, 0:W_], in1=P[bcbank][0:64, 0:W_],
                                                   op=ALU.mult), reads=[ROsb, RP[bcbank]], writes=[Rdst])

        def oproj_tile(Wo, RWo, oT, RoT, ht, Rht, TW, banks):
            for dc in range(8):
                bk = banks[dc % len(banks)]
                for h in range(16):
                    sc.op("pe", lambda e, h=h, dc=dc, bk=bk: e.matmul(
                        P[bk][:, 0:TW], lhsT=Wo[0:64, h, dc * 128:(dc + 1) * 128], rhs=oT[0:64, h, 0:TW],
                        start=(h == 0), stop=(h == 15)), reads=[RWo, RoT], writes=[RP[bk]], inc=(h == 15))
                sc.op("dve", lambda e, dc=dc, bk=bk: e.tensor_tensor(
                    out=ht[:, dc, 0:TW], in0=ht[:, dc, 0:TW], in1=P[bk][:, 0:TW], op=ALU.add),
                    reads=[RP[bk], Rht], writes=[Rht])

        def load_wo(w_o_l, Wo, RWo):
            v = w_o_l.rearrange("(h d) m -> d h m", d=64)
            for h in range(16):
                sc.dma("pool", Wo[0:64, h, :], v[:, h, :], writes=[RWo])

        def phase0():
            with contextlib.ExitStack() as ctx:
                xt = [sb(ctx, "p0_xt%d" % i, [128, 4, D], F32) for i in range(2)]
                ht = [sb(ctx, "p0_ht%d" % i, [128, 8, 512], F32) for i in range(2)]
                Rxt = [Res(), Res()]
                Rht = [Res(), Res()]
                k = 0
                for I in range(NS):
                    b = I % 2
                    sc.dma("sp", xt[b][:, :, :], x[I * 512:(I + 1) * 512, :].rearrange("(j p) d -> p j d", p=128),
                           writes=[Rxt[b]])
                    for c in range(8):
                        bk = k % 4
                        k += 1
                        for j in range(4):
                            sc.op("pe", lambda e, j=j, c=c, bk=bk, b=b: e.transpose(
                                out=P[bk][:, j * 128:(j + 1) * 128], in_=xt[b][:, j, c * 128:(c + 1) * 128],
                                identity=ident_f[:, :]), reads=[Rxt[b], Rc], writes=[RP[bk]], inc=(j == 3))
                        eng = "act" if c % 2 == 0 else "dve"
                        if eng == "act":
                            sc.op("act", lambda e, c=c, bk=bk, b=b: e.copy(out=ht[b][:, c, :], in_=P[bk][:, :]),
                                  reads=[RP[bk]], writes=[Rht[b]])
                        else:
                            sc.op("dve", lambda e, c=c, bk=bk, b=b: e.tensor_copy(out=ht[b][:, c, :], in_=P[bk][:, :]),
                                  reads=[RP[bk]], writes=[Rht[b]])
                    sc.dma("sp", hT_v[:, :, I * 512:(I + 1) * 512], ht[b][:, :, :], reads=[Rht[b]], writes=[RhT])
                sc.barrier()

        def layer_A(l):
            with contextlib.ExitStack() as actx:
                kT = sb(actx, "a_kT", [128, 2, S], BF16)
                vaug = sb(actx, "a_vaug", [128, NT, 4, 65], BF16)
                kiT = sb(actx, "a_kiT", [128, S], BF16)
                wabs = sb(actx, "a_wabs", [128, NT, 8], F32)
                wsgn = sb(actx, "a_wsgn", [128, NT, 8], F32)
                RkT, Rv, Rki, Rw = Res(), Res(), Res(), Res()
                sc.op("pool", lambda e: e.memset(vaug[:, :, :, 64:65], 1.0), writes=[Rv])

                with contextlib.ExitStack() as ctx:
                    Wx = sb(ctx, "a1_Wx", [128, 8, A_IN], BF16)
                    Wsw = sb(ctx, "a1_Wsw", [128, 8, A_IN], BF16)
                    Wki = sb(ctx, "a1_Wki", [128, 8, 128], BF16)
                    Wkisw = sb(ctx, "a1_Wkisw", [128, 8, 128], BF16)
                    RW = Res()
                    wv_ = w_in_a[l].rearrange("(k p) n -> p k n", p=128)
                    for kc in range(8):
                        for (a_, b__, jn) in ((0, 1024, 8), (1024, 1280, 2), (1536, 2048, 4)):
                            for tw in range(2):
                                sc.dma("pool", Wx[:, kc, a_:b__].rearrange("p (j two d) -> p two j d", two=2, d=64)[:, tw],
                                       wv_[:, kc, a_:b__].rearrange("p (two j d) -> p two j d", two=2, d=64)[:, tw],
                                       writes=[RW])
                        sc.dma("pool", Wx[:, kc, 1280:1536], wv_[:, kc, 1280:1536], writes=[RW])
                        sc.dma("pool", Wx[:, kc, 2048:2120], wv_[:, kc, 2048:2120], writes=[RW])
                    kiv = w_in_a[l][:, 2048:2112].rearrange("(k p) n -> p k n", p=128)
                    sc.dma("pool", Wki[:, :, 0:64], kiv, writes=[RW])
                    sc.dma("pool", Wki[:, :, 64:128], kiv, writes=[RW])
                    for (a, b_) in ((0, 1280), (1536, 2112)):
                        src = Wx[:, :, a:b_].rearrange("p k (h two i) -> p k h two i", two=2, i=32)
                        dst = Wsw[:, :, a:b_].rearrange("p k (h two i) -> p k h two i", two=2, i=32)
                        for t in range(2):
                            sc.op("pool", lambda e, src=src, dst=dst, t=t: e.tensor_copy(
                                out=dst[:, :, :, t, :], in_=src[:, :, :, 1 - t, :]), reads=[RW], writes=[RW])
                    srck = Wki[:, :, :].rearrange("p k (h two i) -> p k h two i", two=2, i=32)
                    dstk = Wkisw[:, :, :].rearrange("p k (h two i) -> p k h two i", two=2, i=32)
                    for t in range(2):
                        sc.op("pool", lambda e, t=t: e.tensor_copy(out=dstk[:, :, :, t, :], in_=srck[:, :, :, 1 - t, :]),
                              reads=[RW], writes=[RW])

                    ht = [sb(ctx, "a1_ht%d" % i, [128, 8, 512], F32) for i in range(2)]
                    xn = sb(ctx, "a1_xn", [128, 8, 512], BF16)
                    Ct = [sb(ctx, "a1_C%d" % i, [128, 512], F32) for i in range(2)]
                    St = [sb(ctx, "a1_S%d" % i, [128, 512], F32) for i in range(2)]
                    qt = [sb(ctx, "a1_qt%d" % i, [128, 8, 512], BF16) for i in range(2)]
                    qit = [sb(ctx, "a1_qit%d" % i, [128, 4, 512], BF16) for i in range(2)]
                    t1 = [sb(ctx, "a1_t1%d" % i, [128, 512], F32) for i in range(2)]
                    t2 = [sb(ctx, "a1_t2%d" % i, [128, 512], F32) for i in range(2)]
                    Rht = [Res(), Res()]
                    Rxn = Res()
                    Rtab = [Res(), Res()]
                    Rqt = [Res(), Res()]
                    Rqit = [Res(), Res()]
                    Rt1 = [Res(), Res()]
                    Rt2 = [Res(), Res()]
                    nb = norm_bufs(ctx, 512, 4)

                    def load_tile(I):
                        b = I % 2
                        sc.dma("sp", ht[b][:, :, :], hT_v[:, :, I * 512:(I + 1) * 512], reads=[RhT], writes=[Rht[b]])
                        sc.dma("sp", Ct[b][:, :], c_ropeC[:, I * 512:(I + 1) * 512], writes=[Rtab[b]])
                        sc.dma("sp", St[b][:, :], c_ropeS[:, I * 512:(I + 1) * 512], writes=[Rtab[b]])

                    qv = lambda W, kc: W[:, kc, 0:1024].rearrange("p (j c) -> p j c", c=128)
                    kv_ = lambda W, kc: W[:, kc, 1024:1280].rearrange("p (j c) -> p j c", c=128)
                    qiv = lambda W, kc: W[:, kc, 1536:2048].rearrange("p (j c) -> p j c", c=128)

                    load_tile(0)
                    cn = 0
                    for I in range(NS):
                        b = I % 2
                        if I + 1 < NS:
                            load_tile(I + 1)
                        norm_tile(nb, ht[b], Rht[b], G_A + l, xn, Rxn, 512)
                        chunks = []
                        for j in range(8):
                            chunks.append((lambda kc, j=j: qv(Wx, kc)[:, j], lambda kc, j=j: qv(Wsw, kc)[:, j],
                                           qt[b][:, j, :], Rqt[b]))
                        for j in range(2):
                            chunks.append((lambda kc, j=j: kv_(Wx, kc)[:, j], lambda kc, j=j: kv_(Wsw, kc)[:, j],
                                           kT[:, j, I * 512:(I + 1) * 512], RkT))
                        for j in range(4):
                            chunks.append((lambda kc, j=j: qiv(Wx, kc)[:, j], lambda kc, j=j: qiv(Wsw, kc)[:, j],
                                           qit[b][:, j, :], Rqit[b]))
                        chunks.append((lambda kc: Wki[:, kc, :], lambda kc: Wkisw[:, kc, :],
                                       kiT[:, I * 512:(I + 1) * 512], Rki))
                        for (fa, fb, dst, Rd) in chunks:
                            pb = cn % 2
                            cn += 1
                            pa_, pb_ = 2 * pb, 2 * pb + 1
                            for kc in range(8):
                                sc.op("pe", lambda e, kc=kc, fa=fa, pa_=pa_: e.matmul(
                                    P[pa_][:, :], lhsT=fa(kc), rhs=xn[:, kc, :], start=(kc == 0), stop=(kc == 7)),
                                    reads=[RW, Rxn], writes=[RP[pa_]], inc=(kc == 7))
                            for kc in range(8):
                                sc.op("pe", lambda e, kc=kc, fb=fb, pb_=pb_: e.matmul(
                                    P[pb_][:, :], lhsT=fb(kc), rhs=xn[:, kc, :], start=(kc == 0), stop=(kc == 7)),
                                    reads=[RW, Rxn], writes=[RP[pb_]], inc=(kc == 7))
                            sc.op("dve", lambda e, pa_=pa_, pb=pb, b=b: e.tensor_tensor(
                                out=t1[pb][:, :], in0=P[pa_][:, :], in1=Ct[b][:, :], op=ALU.mult),
                                reads=[RP[pa_], Rtab[b]], writes=[Rt1[pb]])
                            sc.op("dve", lambda e, pb_=pb_, pb=pb, b=b: e.tensor_tensor(
                                out=t2[pb][:, :], in0=P[pb_][:, :], in1=St[b][:, :], op=ALU.mult),
                                reads=[RP[pb_], Rtab[b]], writes=[Rt2[pb]])
                            sc.op("dve", lambda e, dst=dst, pb=pb: e.tensor_tensor(
                                out=dst, in0=t1[pb][:, :], in1=t2[pb][:, :], op=ALU.add),
                                reads=[Rt1[pb], Rt2[pb]], writes=[Rd])
                        for jj in range(4):
                            tile = I * 4 + jj
                            for kc in range(8):
                                sc.op("pe", lambda e, kc=kc, jj=jj: e.matmul(
                                    P[5][:, 0:256], lhsT=xn[:, kc, jj * 128:(jj + 1) * 128], rhs=Wx[:, kc, 1280:1536],
                                    start=(kc == 0), stop=(kc == 7)), reads=[RW, Rxn], writes=[RP[5]], inc=(kc == 7))
                            sc.op("act", lambda e, tile=tile: e.copy(
                                out=vaug[:, tile, :, 0:64], in_=P[5][:, 0:256].rearrange("p (g d) -> p g d", g=4)),
                                reads=[RP[5]], writes=[Rv])
                            for kc in range(8):
                                sc.op("pe", lambda e, kc=kc, jj=jj: e.matmul(
                                    P[6][:, 0:8], lhsT=xn[:, kc, jj * 128:(jj + 1) * 128], rhs=Wx[:, kc, 2112:2120],
                                    start=(kc == 0), stop=(kc == 7)), reads=[RW, Rxn], writes=[RP[6]], inc=(kc == 7))
                            sc.op("act", lambda e, tile=tile: e.activation(out=wabs[:, tile, :], in_=P[6][:, 0:8],
                                                                           func=AF.Abs), reads=[RP[6]], writes=[Rw])
                            sc.op("act", lambda e, tile=tile: e.activation(out=wsgn[:, tile, :], in_=P[6][:, 0:8],
                                                                           func=AF.Sign), reads=[RP[6]], writes=[Rw])
                        RqA = Res()
                        sc.dma("sp", qA.rearrange("(c p) t -> p c t", p=128)[:, :, I * 512:(I + 1) * 512],
                               qt[b][:, :, :], reads=[Rqt[b]], writes=[RqA])
                        sc.dma("sp", qiA.rearrange("(c p) t -> p c t", p=128)[:, :, I * 512:(I + 1) * 512],
                               qit[b][:, :, :], reads=[Rqit[b]], writes=[RqA])
                    sc.barrier()

                with contextlib.ExitStack() as ctx:
                    Wo = sb(ctx, "a2_Wo", [64, 16, D], BF16)
                    RWo = Res()
                    load_wo(w_o_a[l], Wo, RWo)
                    mbA = sb(ctx, "a2_mbA", [128, 2, 256], F32)
                    pw = sb(ctx, "a2_pw", [128, 2 * (NB + 1)], F32)
                    Rk = Res()
                    sc.dma("sp", mbA[:, :, :], c_mbA, writes=[Rk])
                    sc.dma("sp", pw[:, :], c_pw, writes=[Rk])
                    qt = [sb(ctx, "a2_qt%d" % i, [128, 16, 256], BF16) for i in range(2)]
                    qit = [sb(ctx, "a2_qit%d" % i, [128, 4, 256], BF16) for i in range(2)]
                    ht1 = sb(ctx, "a2_ht", [128, 8, 256], F32)
                    ht = [ht1, ht1]
                    score = [sb(ctx, "a2_score%d" % i, [128, S], F32) for i in range(2)]
                    rl = [sb(ctx, "a2_rl%d" % i, [128, 512], F32) for i in range(2)]
                    maskq = [sb(ctx, "a2_mq%d" % i, [128, S], BF16) for i in range(2)]
                    maskT = [sb(ctx, "a2_mT%d" % i, [128, NT, 256], mybir.dt.uint8) for i in range(2)]
                    PT = [sb(ctx, "a2_PT%d" % i, [128, 512], BF16) for i in range(5)]
                    Osb = [sb(ctx, "a2_Osb%d" % i, [128, 512], F32) for i in range(2)]
                    oT = sb(ctx, "a2_oT", [64, 16, 256], BF16)
                    st = [sb(ctx, "a2_st%d" % i, [128, 16], F32) for i in range(2)]
                    wtab = [sb(ctx, "a2_wtab%d" % i, [128, 2 * (NB + 1)], F32) for i in range(2)]
                    midt = [sb(ctx, "a2_midt%d" % i, [128, NB + 2], F32) for i in range(2)]
                    gt = [sb(ctx, "a2_gt%d" % i, [128, NB + 1], F32) for i in range(2)]
                    cnt_t = [sb(ctx, "a2_cnt%d" % i, [128, NB + 1], F32) for i in range(2)]
                    Rqt = [Res(), Res()]
                    Rht1 = Res()
                    Rht = [Rht1, Rht1]
                    Rscore, Rjunk = [Res(), Res()], Res()
                    RS = [Res() for _ in range(4)]
                    Rrl = [Res(), Res()]
                    Rmq = [Res(), Res()]
                    RmT = [Res(), Res()]
                    RPT = [Res() for _ in range(6)]
                    ROsb = [Res(), Res()]
                    Rrden = [Res(), Res()]
                    RoT = Res()
                    Rst = [Res(), Res()]

                    def load_q(T):
                        b = T % 2
                        qAv = qA.rearrange("(c p) t -> p c t", p=128)
                        sc.dma("sp", qt[b][0:64, 0:8, :], qAv[0:64, :, T * 256:(T + 1) * 256], writes=[Rqt[b]])
                        sc.dma("sp", qt[b][64:128, 8:16, :], qAv[64:128, :, T * 256:(T + 1) * 256], writes=[Rqt[b]])
                        sc.dma("sp", qit[b][:, :, :], qiA.rearrange("(c p) t -> p c t", p=128)[:, :, T * 256:(T + 1) * 256],
                               writes=[Rqt[b]])

                    def load_ht(T):
                        sc.dma("sp", ht1[:, :, :], hT_v[:, :, T * 256:(T + 1) * 256], reads=[RhT], writes=[Rht1])

                    for i in range(2):
                        sc.op("pool", lambda e, i=i: e.memset(qt[i][64:128, 0:8, :], 0.0), writes=[Rqt[i]])
                        sc.op("pool", lambda e, i=i: e.memset(qt[i][0:64, 8:16, :], 0.0), writes=[Rqt[i]])
                    load_q(0)
                    ibc = [0]
                    horder = (0, 8, 1, 9, 2, 10, 3, 11, 4, 12, 5, 13, 6, 14, 7, 15)
                    LA = 2
                    NPT = len(PT)
                    mmc = [0]

                    def gen_topk(j, T, b, L):
                        tile = 2 * T + j
                        sco = score[j]
                        Rs = Rscore[j]
                        Rt = Rst[j]
                        st_, wt_, mt_, gt_, ct_ = st[j], wtab[j], midt[j], gt[j], cnt_t[j]
                        for k0 in range(0, L, 512):
                            kw = min(512, L - k0)
                            for hi, h in enumerate((0, 4, 1, 5, 2, 6, 3, 7) if j == 0 else (4, 0, 5, 1, 6, 2, 7, 3)):
                                hb, hc = h // 4, h % 4
                                bk = ibc[0] % 2
                                ibc[0] += 1
                                sc.op("pe", lambda e, hb=hb, hc=hc, bk=bk, k0=k0, kw=kw: e.matmul(
                                    P[bk][:, 0:kw], lhsT=qit[b][hb * 64:(hb + 1) * 64, hc, j * 128:(j + 1) * 128],
                                    rhs=kiT[hb * 64:(hb + 1) * 64, k0:k0 + kw], start=True, stop=True),
                                    reads=[Rqt[b], Rki], writes=[RP[bk]])
                                sc.op("act", lambda e, bk=bk, kw=kw, h=h: e.activation(
                                    out=rl[bk][:, 0:kw], in_=P[bk][:, 0:kw], func=AF.Relu,
                                    scale=wabs[:, tile, h:h + 1]), reads=[RP[bk], Rw], writes=[Rrl[bk]])
                                if hi == 0:
                                    sc.op("dve", lambda e, bk=bk, kw=kw, k0=k0, h=h: e.tensor_scalar(
                                        out=sco[:, k0:k0 + kw], in0=rl[bk][:, 0:kw], scalar1=wsgn[:, tile, h:h + 1],
                                        scalar2=None, op0=ALU.mult), reads=[Rrl[bk], Rw], writes=[Rs])
                                else:
                                    sc.op("dve", lambda e, bk=bk, kw=kw, k0=k0, h=h: e.scalar_tensor_tensor(
                                        out=sco[:, k0:k0 + kw], in0=rl[bk][:, 0:kw], scalar=wsgn[:, tile, h:h + 1],
                                        in1=sco[:, k0:k0 + kw], op0=ALU.mult, op1=ALU.add),
                                        reads=[Rrl[bk], Rw, Rs], writes=[Rs])
                                yield
                        sc.op("dve", lambda e: e.tensor_reduce(out=st_[:, 0:1], in_=sco[:, 0:L], axis=AX.X, op=ALU.min),
                              reads=[Rs], writes=[Rt])
                        sc.op("dve", lambda e: e.tensor_reduce(out=st_[:, 1:2], in_=sco[:, 0:L], axis=AX.X, op=ALU.max),
                              reads=[Rs], writes=[Rt])
                        yield
                        sc.op("dve", lambda e: e.tensor_tensor(out=sco[:, L - 256:L], in0=sco[:, L - 256:L],
                                                               in1=mbA[:, j, :], op=ALU.add),
                              reads=[Rs, Rk, Rt], writes=[Rs])
                        sc.op("dve", lambda e: e.tensor_tensor(out=st_[:, 2:3], in0=st_[:, 1:2], in1=st_[:, 0:1],
                                                               op=ALU.subtract), reads=[Rt], writes=[Rt])
                        yield
                        sc.op("dve", lambda e: e.tensor_scalar(out=wt_[:, :], in0=pw[:, :], scalar1=st_[:, 2:3],
                                                               scalar2=None, op0=ALU.mult), reads=[Rt, Rk], writes=[Rt])
                        yield
                        sc.op("dve", lambda e: e.tensor_tensor(out=mt_[:, 0:1], in0=st_[:, 0:1], in1=wt_[:, 0:1],
                                                               op=ALU.add), reads=[Rt], writes=[Rt])
                        yield
                        for n in range(NB):
                            if j == 0:
                                sc.op("dve", lambda e, n=n: e.tensor_scalar(
                                    out=maskq[j][:, 0:L], in0=sco[:, 0:L], scalar1=mt_[:, n:n + 1], scalar2=None,
                                    op0=ALU.is_ge, op1=ALU.add, accum_out=ct_[:, n:n + 1]),
                                    reads=[Rs, Rt], writes=[Rmq[j], Rt])
                                yield
                                sc.op("dve", lambda e, n=n: e.tensor_scalar(
                                    out=gt_[:, n:n + 1], in0=ct_[:, n:n + 1], scalar1=float(TOPK) - 0.5,
                                    scalar2=wt_[:, NB + 1 + n + 1:NB + 1 + n + 2], op0=ALU.is_ge, op1=ALU.mult),
                                    reads=[Rt], writes=[Rt])
                            else:
                                sc.op("act", lambda e, n=n: e.activation(
                                    out=maskq[j][:, 0:L], in_=sco[:, 0:L], func=AF.Sign, bias=mt_[:, n:n + 1], scale=-1.0,
                                    accum_out=ct_[:, n:n + 1]), reads=[Rs, Rt], writes=[Rmq[j], Rt])
                                yield
                                sc.op("dve", lambda e, n=n: e.tensor_scalar(
                                    out=gt_[:, n:n + 1], in0=ct_[:, n:n + 1], scalar1=float(L - 2 * TOPK + 1),
                                    scalar2=wt_[:, NB + 1 + n + 1:NB + 1 + n + 2], op0=ALU.is_le, op1=ALU.mult),
                                    reads=[Rt], writes=[Rt])
                            yield
                            sc.op("dve", lambda e, n=n: e.scalar_tensor_tensor(
                                out=mt_[:, n + 1:n + 2], in0=mt_[:, n:n + 1], scalar=wt_[:, n + 1:n + 2],
                                in1=gt_[:, n:n + 1], op0=ALU.subtract, op1=ALU.add), reads=[Rt], writes=[Rt])
                            yield
                        sc.op("dve", lambda e: e.tensor_tensor(out=mt_[:, NB + 1:NB + 2], in0=mt_[:, NB:NB + 1],
                                                               in1=wt_[:, NB:NB + 1], op=ALU.subtract),
                              reads=[Rt], writes=[Rt])
                        yield
                        sc.op("dve", lambda e: e.tensor_scalar(
                            out=maskq[j][:, 0:L], in0=sco[:, 0:L], scalar1=mt_[:, NB + 1:NB + 2], scalar2=None,
                            op0=ALU.is_ge), reads=[Rs, Rt], writes=[Rmq[j]])
                        yield


                    def gen_X(T):
                        b = T % 2
                        L = 256 * (T + 1)
                        nch = L // 128
                        alive = [gen_topk(0, T, b, L), gen_topk(1, T, b, L)]
                        while alive:
                            for g_ in list(alive):
                                try:
                                    next(g_)
                                    yield
                                except StopIteration:
                                    alive.remove(g_)
                        mT = maskT[b]
                        for c0 in range(0, nch, 4):
                            cw = min(4, nch - c0)
                            for cc in range(cw):
                                for j in range(2):
                                    last = (cc == cw - 1 and j == 1)
                                    sc.op("pe", lambda e, cc=cc, j=j, c0=c0: e.transpose(
                                        out=PB[:, cc * 256 + j * 128:cc * 256 + (j + 1) * 128],
                                        in_=maskq[j][:, (c0 + cc) * 128:(c0 + cc + 1) * 128], identity=ident_b[:, :]),
                                        reads=[Rmq[j], Rc], writes=[RPB], inc=last)
                            sc.op("act", lambda e, c0=c0, cw=cw: e.copy(
                                out=mT[:, c0:c0 + cw, :], in_=PB[:, 0:cw * 256].rearrange("p (c t) -> p c t", t=256)),
                                reads=[RPB], writes=[RmT[b]])
                            yield

                    def gen_Y(T):
                        b = T % 2
                        L = 256 * (T + 1)
                        nch = L // 128
                        mT = maskT[b]
                        items = [(g, c, p) for g in (0, 2, 1, 3) for c in range(nch) for p in range(2)]

                        def front(i):
                            g, c, p = items[i]
                            hb = g // 2
                            kj = g % 2
                            c0 = 4 * (g % 2) + 2 * p
                            sl = 2 + i % 2
                            pk = i % NPT
                            sc.op("pe", lambda e: e.matmul(
                                P[sl][:, :], lhsT=kT[:, kj, c * 128:(c + 1) * 128],
                                rhs=qt[b][:, hb * 8 + c0:hb * 8 + c0 + 2, :].rearrange("p c t -> p (c t)"),
                                start=True, stop=True), reads=[RkT, Rqt[b]], writes=[RP[sl]])
                            sc.op("act", lambda e: e.activation(
                                out=PT[pk][:, :], in_=P[sl][:, :], func=AF.Exp, scale=0.125),
                                reads=[RP[sl]], writes=[RPT[pk]])
                            mmc[0] += 1
                            meng = "pool" if (mmc[0] % 3 == 0) else "dve"
                            mbase = mT[:, c, :]
                            mbc = bass.AP(mbase.tensor, mbase.offset, [list(mbase.ap[0]), [0, 2], list(mbase.ap[1])])
                            ptv = PT[pk][:, :].rearrange("p (a t) -> p a t", a=2)
                            sc.op(meng, lambda e: e.tensor_tensor(out=ptv, in0=ptv, in1=mbc, op=ALU.mult),
                                  reads=[RPT[pk], RmT[b]], writes=[RPT[pk]])

                        def back(i):
                            g, c, p = items[i]
                            ob = 4 + p
                            pk = i % NPT
                            sc.op("pe", lambda e: e.matmul(
                                P[ob][0:65, :], lhsT=vaug[:, c, g, :], rhs=PT[pk][:, :],
                                start=(c == 0), stop=(c == nch - 1)), reads=[RPT[pk], Rv], writes=[RP[ob]],
                                inc=(c == nch - 1))
                            if c == nch - 1:
                                h0 = 4 * g + 2 * p
                                normalize_heads(P[ob], RP[ob], 512, Osb[p], ROsb[p], None, None,
                                                oT[0:64, h0:h0 + 2, :].rearrange("p a t -> p (a t)"), RoT, 6,
                                                use_act=True)

                        for i in range(len(items) + LA):
                            if i < len(items):
                                front(i)
                            if i - LA >= 0:
                                back(i - LA)
                            yield
                        oproj_tile(Wo, RWo, oT, RoT, ht[b], Rht[b], 256, (2, 3))
                        sc.dma("sp", hT_v[:, :, T * 256:(T + 1) * 256], ht[b][:, :, :], reads=[Rht[b]], writes=[RhT])
                        yield

                    def nsteps_X(T):
                        L = 256 * (T + 1)
                        return 2 * (((L + 511) // 512) * 8 + 6 + 3 * NB + 2) + (L // 128 + 3) // 4

                    def nsteps_Y(T):
                        return 8 * (256 * (T + 1) // 128) + LA + 1

                    for _ in gen_X(0):
                        pass
                    for T in range(NQ):
                        if T + 1 < NQ:
                            load_q(T + 1)
                        load_ht(T)
                        gy = gen_Y(T)
                        gx = gen_X(T + 1) if T + 1 < NQ else None
                        ratio = (nsteps_X(T + 1) / float(nsteps_Y(T))) if gx is not None else 0.0
                        acc = 0.0
                        for _ in gy:
                            if gx is not None:
                                acc += ratio
                                while acc >= 1.0 and gx is not None:
                                    acc -= 1.0
                                    try:
                                        next(gx)
                                    except StopIteration:
                                        gx = None
                        if gx is not None:
                            for _ in gx:
                                pass
                    sc.barrier()

        def phase_mlp(layer):
            with contextlib.ExitStack() as ctx:
                Wup = sb(ctx, "m_Wup", [128, 8, DFF], BF16)
                Wdn = sb(ctx, "m_Wdn", [128, 32, D], BF16)
                RWu = [Res() for _ in range(4)]
                RWd = [Res() for _ in range(4)]
                wuv = w_up[layer].rearrange("(k p) n -> p k n", p=128)
                wdv = w_down[layer].rearrange("(k p) n -> p k n", p=128)
                for fb in range(4):
                    for k in range(8):
                        sc.dma("pool", Wup[:, k, fb * 1024:(fb + 1) * 1024], wuv[:, k, fb * 1024:(fb + 1) * 1024],
                               writes=[RWu[fb]])
                for fb in range(4):
                    for k in range(8 * fb, 8 * fb + 8):
                        sc.dma("pool", Wdn[:, k, :], wdv[:, k, :], writes=[RWd[fb]])
                TW = 256
                ht = [sb(ctx, "m_ht%d" % i, [128, 8, TW], F32) for i in range(2)]
                xn = [sb(ctx, "m_xn%d" % i, [128, 8, TW], BF16) for i in range(2)]
                rl = [sb(ctx, "m_rl%d" % i, [128, TW], F32) for i in range(4)]
                act = [sb(ctx, "m_act%d" % i, [128, 32, TW], BF16) for i in range(2)]
                Rht = [Res(), Res()]
                Rxn = [Res(), Res()]
                Rrl = [Res() for _ in range(4)]
                Ract = [Res(), Res()]
                nb = norm_bufs(ctx, TW, 6)
                sc.dma("sp", ht[0][:, :, :], hT_v[:, :, 0:TW], reads=[RhT], writes=[Rht[0]])
                uk = 0
                for T in range(S // TW):
                    b = T % 2
                    if T + 1 < S // TW:
                        sc.dma("sp", ht[1 - b][:, :, :], hT_v[:, :, (T + 1) * TW:(T + 2) * TW], reads=[RhT],
                               writes=[Rht[1 - b]])
                    norm_tile(nb, ht[b], Rht[b], G_M + layer, xn[b], Rxn[b], TW)
                    for fc in range(32):
                        bk = uk % 4
                        uk += 1
                        for kc in range(8):
                            sc.op("pe", lambda e, kc=kc, fc=fc, bk=bk, b=b: e.matmul(
                                P[bk][:, 0:TW], lhsT=Wup[:, kc, fc * 128:(fc + 1) * 128], rhs=xn[b][:, kc, :],
                                start=(kc == 0), stop=(kc == 7)), reads=[RWu[fc // 8], Rxn[b]], writes=[RP[bk]], inc=(kc == 7))
                        sc.op("act", lambda e, bk=bk: e.activation(out=rl[bk][:, :], in_=P[bk][:, 0:TW], func=AF.Relu),
                              reads=[RP[bk]], writes=[Rrl[bk]])
                        meng = "dve"
                        sc.op(meng, lambda e, bk=bk, fc=fc, b=b: e.tensor_tensor(
                            out=act[b][:, fc, :], in0=rl[bk][:, :], in1=rl[bk][:, :], op=ALU.mult),
                            reads=[Rrl[bk]], writes=[Ract[b]])
                    for dc in range(8):
                        bk = 4 + dc % 2
                        for fc in range(32):
                            sc.op("pe", lambda e, fc=fc, dc=dc, bk=bk, b=b: e.matmul(
                                P[bk][:, 0:TW], lhsT=Wdn[:, fc, dc * 128:(dc + 1) * 128], rhs=act[b][:, fc, :],
                                start=(fc == 0), stop=(fc == 31)), reads=[RWd[fc // 8], Ract[b]], writes=[RP[bk]], inc=(fc == 31))
                        sc.op("dve", lambda e, dc=dc, bk=bk, b=b: e.tensor_tensor(
                            out=ht[b][:, dc, :], in0=ht[b][:, dc, :], in1=P[bk][:, 0:TW], op=ALU.add),
                            reads=[RP[bk], Rht[b]], writes=[Rht[b]])
                    sc.dma("sp", hT_v[:, :, T * TW:(T + 1) * TW], ht[b][:, :, :], reads=[Rht[b]], writes=[RhT])
                sc.barrier()

        def phase_kv_full():
            octx = contextlib.ExitStack()
            flog = sb(octx, "kv_flog", [16, S], F32)
            Rfl = Res()
            Rkaug, Rqaug, RvB = Res(), Res(), Res()
            with contextlib.ExitStack() as ctx:
                Wkv = sb(ctx, "kv_W", [128, 8, KV_IN], BF16)
                RW = Res()
                load_w(w_kv_b, Wkv, 8, RW)
                ht = [sb(ctx, "kv_ht%d" % i, [128, 8, 512], F32) for i in range(2)]
                xn = sb(ctx, "kv_xn", [128, 8, 512], BF16)
                kst = [sb(ctx, "kv_kst%d" % i, [128, 8, 512], BF16) for i in range(2)]
                vst = [sb(ctx, "kv_vst%d" % i, [128, 4, 16, 65], BF16) for i in range(2)]
                Rht = [Res(), Res()]
                Rxn = Res()
                Rkst = [Res(), Res()]
                Rvst = [Res(), Res()]
                nb = norm_bufs(ctx, 512, 6)
                for i in range(2):
                    sc.op("pool", lambda e, i=i: e.memset(vst[i][:, :, :, 64:65], 1.0), writes=[Rvst[i]])
                sc.dma("sp", ht[0][:, :, :], hT_v[:, :, 0:512], reads=[RhT], writes=[Rht[0]])
                kaug_v = kaug.rearrange("(c b) r t -> b r c t", b=2)
                pk = 0
                for I in range(NS):
                    b = I % 2
                    if I + 1 < NS:
                        sc.dma("sp", ht[1 - b][:, :, :], hT_v[:, :, (I + 1) * 512:(I + 2) * 512], reads=[RhT],
                               writes=[Rht[1 - b]])
                    norm_tile(nb, ht[b], Rht[b], G_KV, xn, Rxn, 512)
                    for c in range(8):
                        bk = pk % 4
                        pk += 1
                        for kc in range(8):
                            sc.op("pe", lambda e, kc=kc, c=c, bk=bk: e.matmul(
                                P[bk][:, :], lhsT=Wkv[:, kc, c * 128:(c + 1) * 128], rhs=xn[:, kc, :],
                                start=(kc == 0), stop=(kc == 7)), reads=[RW, Rxn], writes=[RP[bk]], inc=(kc == 7))
                        sc.op("act", lambda e, c=c, bk=bk, b=b: e.copy(out=kst[b][:, c, :], in_=P[bk][:, :]),
                              reads=[RP[bk]], writes=[Rkst[b]])
                    for hb in range(2):
                        sc.dma("sp", kaug_v[hb, 0:64, :, I * 512:(I + 1) * 512], kst[b][hb * 64:(hb + 1) * 64, :, :],
                               reads=[Rkst[b]], writes=[Rkaug])
                    for jj in range(4):
                        for half in range(2):
                            bk = pk % 4
                            pk += 1
                            for kc in range(8):
                                sc.op("pe", lambda e, kc=kc, jj=jj, half=half, bk=bk: e.matmul(
                                    P[bk][:, :], lhsT=xn[:, kc, jj * 128:(jj + 1) * 128],
                                    rhs=Wkv[:, kc, 1024 + half * 512:1024 + (half + 1) * 512],
                                    start=(kc == 0), stop=(kc == 7)), reads=[RW, Rxn], writes=[RP[bk]], inc=(kc == 7))
                            sc.op("act", lambda e, jj=jj, half=half, bk=bk, b=b: e.copy(
                                out=vst[b][:, jj, half * 8:(half + 1) * 8, 0:64],
                                in_=P[bk][:, :].rearrange("p (h d) -> p h d", d=64)), reads=[RP[bk]], writes=[Rvst[b]])
                    sc.dma("sp", vB[I * 512:(I + 1) * 512].rearrange("(j p) h e -> p j h e", p=128), vst[b][:, :, :, :],
                           reads=[Rvst[b]], writes=[RvB])
                    for kc in range(8):
                        sc.op("pe", lambda e, kc=kc: e.matmul(
                            P[4][0:16, :], lhsT=Wkv[:, kc, 2048:2064], rhs=xn[:, kc, :],
                            start=(kc == 0), stop=(kc == 7)), reads=[RW, Rxn], writes=[RP[4]], inc=(kc == 7))
                    sc.op("act", lambda e, I=I: e.copy(out=flog[0:16, I * 512:(I + 1) * 512], in_=P[4][0:16, :]),
                          reads=[RP[4]], writes=[Rfl])
                sc.barrier()
            with contextlib.ExitStack() as ctx:
                ex = sb(ctx, "kv_ex", [16, S], F32)
                onesS = sb(ctx, "kv_ones", [16, S], F32)
                cum = sb(ctx, "kv_cum", [16, S], F32)
                qrows = sb(ctx, "kv_qrows", [16, 3, S], BF16)
                krows = sb(ctx, "kv_krows", [16, 3, S], BF16)
                orows = sb(ctx, "kv_orows", [16, 3, S], BF16)
                nbf = sb(ctx, "kv_nbf", [16, 1], F32)
                Rm = Res()
                sc.op("pool", lambda e: e.memset(onesS[:, :], 1.0), writes=[Rm])
                sc.op("pool", lambda e: e.memset(orows[:, :, :], 1.0), writes=[Rm])
                sc.dma("sp", nbf[:, :], b_f.rearrange("(p o) -> p o", o=1), writes=[Rm])
                sc.op("dve", lambda e: e.tensor_scalar(out=nbf[:, :], in0=nbf[:, :], scalar1=-1.0, scalar2=None,
                                                       op0=ALU.mult), reads=[Rm], writes=[Rm])
                sc.op("act", lambda e: e.activation(out=ex[:, :], in_=flog[:, :], func=AF.Exp, bias=nbf[:, :], scale=-1.0),
                      reads=[Rfl, Rm], writes=[Rm])
                sc.op("act", lambda e: e.activation(out=ex[:, :], in_=ex[:, :], func=AF.Ln, bias=one_t[0:16, :], scale=1.0),
                      reads=[Rm, Rc], writes=[Rm])
                sc.op("dve", lambda e: e.tensor_tensor_scan(out=cum[:, :], data0=onesS[:, :], data1=ex[:, :], initial=0.0,
                                                            op0=ALU.mult, op1=ALU.subtract), reads=[Rm], writes=[Rm])
                sc.op("dve", lambda e: e.tensor_scalar(out=cum[:, :], in0=cum[:, :], scalar1=8.0, scalar2=None,
                                                       op0=ALU.mult), reads=[Rm], writes=[Rm])
                for r in range(3):
                    sc.op("dve", lambda e, r=r: e.tensor_copy(out=qrows[:, r, :], in_=cum[:, :]), reads=[Rm], writes=[Rm])
                    if r < 2:
                        sc.op("dve", lambda e, r=r: e.tensor_tensor(out=cum[:, :], in0=cum[:, :], in1=qrows[:, r, :],
                                                                    op=ALU.subtract), reads=[Rm], writes=[Rm])
                sc.op("dve", lambda e: e.tensor_scalar(out=krows[:, :, :], in0=qrows[:, :, :], scalar1=-1.0, scalar2=None,
                                                       op0=ALU.mult), reads=[Rm], writes=[Rm])
                sc.dma("sp", qaug[:, 64:67, :], qrows[:, :, :], reads=[Rm], writes=[Rqaug])
                sc.dma("sp", qaug[:, 67:70, :], orows[:, :, :], reads=[Rm], writes=[Rqaug])
                sc.dma("sp", kaug[:, 64:67, :], orows[:, :, :], reads=[Rm], writes=[Rkaug])
                sc.dma("sp", kaug[:, 67:70, :], krows[:, :, :], reads=[Rm], writes=[Rkaug])
                sc.barrier()
            octx.close()

        def layer_B(l):
            with contextlib.ExitStack() as ctx:
                Wq = sb(ctx, "b1_W", [128, 8, D], BF16)
                RW = Res()
                load_w(w_q_b[l], Wq, 8, RW)
                ht = [sb(ctx, "b1_ht%d" % i, [128, 8, 512], F32) for i in range(2)]
                xn = sb(ctx, "b1_xn", [128, 8, 512], BF16)
                qst = [sb(ctx, "b1_qst%d" % i, [128, 8, 512], BF16) for i in range(2)]
                Rht = [Res(), Res()]
                Rxn = Res()
                Rqst = [Res(), Res()]
                Rq = Res()
                nb = norm_bufs(ctx, 512, 6)
                qaug_v = qaug.rearrange("(c b) r t -> b r c t", b=2)
                sc.dma("sp", ht[0][:, :, :], hT_v[:, :, 0:512], reads=[RhT], writes=[Rht[0]])
                pk = 0
                for I in range(NS):
                    b = I % 2
                    if I + 1 < NS:
                        sc.dma("sp", ht[1 - b][:, :, :], hT_v[:, :, (I + 1) * 512:(I + 2) * 512], reads=[RhT],
                               writes=[Rht[1 - b]])
                    norm_tile(nb, ht[b], Rht[b], G_B + l, xn, Rxn, 512)
                    for c in range(8):
                        bk = pk % 4
                        pk += 1
                        for kc in range(8):
                            sc.op("pe", lambda e, kc=kc, c=c, bk=bk: e.matmul(
                                P[bk][:, :], lhsT=Wq[:, kc, c * 128:(c + 1) * 128], rhs=xn[:, kc, :],
                                start=(kc == 0), stop=(kc == 7)), reads=[RW, Rxn], writes=[RP[bk]], inc=(kc == 7))
                        sc.op("act", lambda e, c=c, bk=bk, b=b: e.copy(out=qst[b][:, c, :], in_=P[bk][:, :]),
                              reads=[RP[bk]], writes=[Rqst[b]])
                    for hb in range(2):
                        sc.dma("sp", qaug_v[hb, 0:64, :, I * 512:(I + 1) * 512], qst[b][hb * 64:(hb + 1) * 64, :, :],
                               reads=[Rqst[b]], writes=[Rq])
                sc.barrier()
            with contextlib.ExitStack() as ctx:
                kh = [sb(ctx, "b2_kh%d" % i, [128, S], BF16) for i in range(2)]
                qh = [sb(ctx, "b2_qh%d" % i, [128, S], BF16) for i in range(2)]
                vh = [sb(ctx, "b2_vh%d" % i, [128, NT, 65], BF16) for i in range(2)]
                cb = sb(ctx, "b2_cb", [128, 4, 512], F32)
                PT = [sb(ctx, "b2_PT%d" % i, [128, 512], BF16) for i in range(6)]
                stmp = [sb(ctx, "b2_st%d" % i, [128, 512], F32) for i in range(3)]
                Osb = [sb(ctx, "b2_Osb%d" % i, [128, 512], F32) for i in range(2)]
                rden = [sb(ctx, "b2_rden%d" % i, [128, 512], F32) for i in range(2)]
                oTh = [sb(ctx, "b2_oTh%d" % i, [64, 512], BF16) for i in range(2)]
                Rh = [Res(), Res()]
                Rcb = Res()
                RPT = [Res() for _ in range(6)]
                Rstmp = [Res(), Res(), Res()]
                ROsb = [Res(), Res()]
                Rrden = [Res(), Res()]
                RoTh = [Res(), Res()]
                Ro = Res()
                sc.dma("sp", cb[:, :, :], c_cbB, writes=[Rcb])

                def load_head(h):
                    hb_ = h % 2
                    sc.dma("sp", kh[hb_][0:70, :], kaug[h], writes=[Rh[hb_]])
                    sc.dma("sp", qh[hb_][0:70, :], qaug[h], writes=[Rh[hb_]])
                    sc.dma("sp", vh[hb_][:, :, :], vB[:, h, :].rearrange("(c p) e -> p c e", p=128), writes=[Rh[hb_]])

                load_head(0)
                load_head(1)
                items = []
                ti = 0
                for h in range(16):
                    for T in range(NS):
                        nch = 4 * (T + 1)
                        for c in range(nch):
                            items.append((h, T, c, nch, ti))
                        ti += 1
                LA = 3
                NPT = len(PT)
                dkc = [0]

                def front(i):
                    h, T, c, nch, ti_ = items[i]
                    hb_ = h % 2
                    sk = i % 4
                    pk = i % NPT
                    sc.op("pe", lambda e: e.matmul(
                        P[sk][:, :], lhsT=kh[hb_][0:70, c * 128:(c + 1) * 128],
                        rhs=qh[hb_][0:70, T * 512:(T + 1) * 512], start=True, stop=True),
                        reads=[Rh[hb_]], writes=[RP[sk]])
                    if c >= 4 * T:
                        d_ = dkc[0] % len(stmp)
                        dkc[0] += 1
                        cc = c - 4 * T
                        sc.op("dve", lambda e: e.tensor_tensor(
                            out=stmp[d_][:, :], in0=P[sk][:, :], in1=cb[:, cc, :], op=ALU.add),
                            reads=[RP[sk], Rcb], writes=[Rstmp[d_]])
                        sc.op("act", lambda e: e.activation(
                            out=PT[pk][:, :], in_=stmp[d_][:, :], func=AF.Exp, scale=0.125),
                            reads=[Rstmp[d_]], writes=[RPT[pk]])
                    else:
                        sc.op("act", lambda e: e.activation(
                            out=PT[pk][:, :], in_=P[sk][:, :], func=AF.Exp, scale=0.125),
                            reads=[RP[sk]], writes=[RPT[pk]])

                def back(i):
                    h, T, c, nch, ti_ = items[i]
                    hb_ = h % 2
                    pk = i % NPT
                    ob = 4 + ti_ % 2
                    ob_i = ti_ % 2
                    sc.op("pe", lambda e: e.matmul(
                        P[ob][0:65, :], lhsT=vh[hb_][:, c, :], rhs=PT[pk][:, :],
                        start=(c == 0), stop=(c == nch - 1)), reads=[RPT[pk], Rh[hb_]], writes=[RP[ob]],
                        inc=(c == nch - 1))
                    if c == nch - 1:
                        normalize_heads(P[ob], RP[ob], 512, Osb[ob_i], ROsb[ob_i], rden[ob_i], Rrden[ob_i],
                                        oTh[ob_i][:, :], RoTh[ob_i], 6)
                        sc.dma("sp", oTs[h, :, T * 512:(T + 1) * 512], oTh[ob_i][:, :], reads=[RoTh[ob_i]], writes=[Ro])
                        if T == NS - 1 and h + 2 < 16:
                            load_head(h + 2)

                for i in range(len(items) + LA):
                    if i < len(items):
                        front(i)
                    if i - LA >= 0:
                        back(i - LA)
                sc.barrier()
            with contextlib.ExitStack() as ctx:
                Wo = sb(ctx, "b3_Wo", [64, 16, D], BF16)
                RWo = Res()
                load_wo(w_o_b[l], Wo, RWo)
                ht = [sb(ctx, "b3_ht%d" % i, [128, 8, 512], F32) for i in range(2)]
                oT = [sb(ctx, "b3_oT%d" % i, [64, 16, 512], BF16) for i in range(2)]
                Rht = [Res(), Res()]
                RoT = [Res(), Res()]
                oTs_v = oTs.rearrange("h d t -> d h t")

                def ld(I):
                    b = I % 2
                    sc.dma("sp", ht[b][:, :, :], hT_v[:, :, I * 512:(I + 1) * 512], reads=[RhT], writes=[Rht[b]])
                    sc.dma("sp", oT[b][:, :, :], oTs_v[:, :, I * 512:(I + 1) * 512], writes=[RoT[b]])

                ld(0)
                for I in range(NS):
                    b = I % 2
                    if I + 1 < NS:
                        ld(I + 1)
                    oproj_tile(Wo, RWo, oT[b], RoT[b], ht[b], Rht[b], 512, (0, 1, 2, 3))
                    sc.dma("sp", hT_v[:, :, I * 512:(I + 1) * 512], ht[b][:, :, :], reads=[Rht[b]], writes=[RhT])
                sc.barrier()

        def phase_final(dump_raw=False):
            with contextlib.ExitStack() as ctx:
                ht = [sb(ctx, "f_ht%d" % i, [128, 8, 512], F32) for i in range(2)]
                xn = [sb(ctx, "f_xn%d" % i, [128, 8, 512], F32) for i in range(2)]
                ot = [sb(ctx, "f_ot%d" % i, [128, 4, D], F32) for i in range(2)]
                Rht = [Res(), Res()]
                Rxn = [Res(), Res()]
                Rot = [Res(), Res()]
                Rout = Res()
                nb = norm_bufs(ctx, 512, 6)
                sc.dma("sp", ht[0][:, :, :], hT_v[:, :, 0:512], reads=[RhT], writes=[Rht[0]])
                k = 0
                for I in range(NS):
                    b = I % 2
                    if I + 1 < NS:
                        sc.dma("sp", ht[1 - b][:, :, :], hT_v[:, :, (I + 1) * 512:(I + 2) * 512], reads=[RhT],
                               writes=[Rht[1 - b]])
                    if dump_raw:
                        src, Rsrc = ht[b], Rht[b]
                    else:
                        norm_tile(nb, ht[b], Rht[b], G_F, xn[b], Rxn[b], 512, final=True)
                        src, Rsrc = xn[b], Rxn[b]
                    for j in range(4):
                        for cg in range(2):
                            bk = k % 4
                            k += 1
                            for cc in range(4):
                                c = cg * 4 + cc
                                sc.op("pe", lambda e, j=j, c=c, cc=cc, bk=bk, src=src: e.transpose(
                                    out=P[bk][:, cc * 128:(cc + 1) * 128], in_=src[:, c, j * 128:(j + 1) * 128],
                                    identity=ident_f[:, :]), reads=[Rsrc, Rc], writes=[RP[bk]], inc=(cc == 3))
                            if (j * 2 + cg) % 2 == 0:
                                sc.op("act", lambda e, j=j, cg=cg, bk=bk, b=b: e.copy(
                                    out=ot[b][:, j, cg * 512:(cg + 1) * 512], in_=P[bk][:, :]),
                                    reads=[RP[bk]], writes=[Rot[b]])
                            else:
                                sc.op("dve", lambda e, j=j, cg=cg, bk=bk, b=b: e.tensor_copy(
                                    out=ot[b][:, j, cg * 512:(cg + 1) * 512], in_=P[bk][:, :]),
                                    reads=[RP[bk]], writes=[Rot[b]])
                    sc.dma("sp", out[I * 512:(I + 1) * 512, :].rearrange("(j p) d -> p j d", p=128), ot[b][:, :, :],
                           reads=[Rot[b]], writes=[Rout])
                sc.barrier()

        plan = ["p0", "A0", "M0", "A1", "M1", "KV", "B0", "M2", "B1", "M3"]
        phase0()
        done = False
        for ph in plan[1:]:
            if stop is not None and plan.index(ph) > plan.index(stop):
                done = True
                break
            if ph[0] == "A":
                layer_A(int(ph[1]))
            elif ph[0] == "M":
                phase_mlp(int(ph[1]))
            elif ph == "KV":
                phase_kv_full()
            elif ph[0] == "B":
                layer_B(int(ph[1]))
        phase_final(dump_raw=(stop is not None))
    return nc


def make_consts(S):
    ident = np.eye(128, dtype=np.float32)
    d = 64
    inv = (1.0 / (np.float32(10000.0) ** (np.arange(0, d, 2, dtype=np.float32) / np.float32(d)))).astype(np.float32)
    ang = (np.arange(S, dtype=np.float32)[:, None] * inv[None, :]).astype(np.float32)
    cs = np.cos(ang).astype(np.float32).T
    sn = np.sin(ang).astype(np.float32).T
    p = np.arange(128)
    ropeC = cs[p % 32, :]
    sign = np.where((p % 64) < 32, -1.0, 1.0).astype(np.float32)
    ropeS = sn[p % 32, :] * sign[:, None]
    mbA = np.zeros((128, 2, 256), np.float32)
    r = np.arange(128)[:, None]
    s = np.arange(256)[None, :]
    for j in range(2):
        mbA[:, j, :] = np.where(s >= 128 * j + 64 * (r // 64 + 1), -BIG, 0.0)
    cbB = np.zeros((128, 4, 512), np.float32)
    t = np.arange(512)[None, :]
    for cc in range(4):
        cbB[:, cc, :] = np.where((128 * cc + r) > t, -1.0e9, 0.0)
    pw1 = np.array([2.0 ** -(n + 1) for n in range(NB + 1)], np.float32)
    pw = np.concatenate([pw1, 2 * pw1])[None, :].repeat(128, 0).astype(np.float32)
    return {"c_ident": ident, "c_ropeC": np.ascontiguousarray(ropeC, np.float32),
            "c_ropeS": np.ascontiguousarray(ropeS, np.float32), "c_mbA": mbA, "c_cbB": cbB, "c_pw": pw}


_NC_CACHE = {}


def kernel(**inputs):
    S = inputs["x"].shape[1]
    B = inputs["x"].shape[0]
    if S not in _NC_CACHE:
        _NC_CACHE[S] = build(S)
    nc = _NC_CACHE[S]
    consts = make_consts(S)
    in_maps = []
    for b in range(B):
        m = {k: np.ascontiguousarray(np.asarray(v, dtype=np.float32)) for k, v in inputs.items() if k != "x"}
        m["x"] = np.ascontiguousarray(np.asarray(inputs["x"][b], dtype=np.float32))
        m.update(consts)
        in_maps.append(m)
    res = run_bass_kernel_spmd(nc, in_maps, core_ids=list(range(B)))
    return np.stack([np.asarray(r["out"], dtype=np.float32) for r in res.results], axis=0)
```

```python
import contextlib
import numpy as np
import concourse.bass as bass
import concourse.mybir as mybir
from concourse.bass_utils import run_bass_kernel_spmd

F32 = mybir.dt.float32
BF16 = mybir.dt.bfloat16
AF = mybir.ActivationFunctionType
ALU = mybir.AluOpType
AX = mybir.AxisListType

D = 1024
DFF = 4096
A_IN = 2120
KV_IN = 2064
EPS = 1e-6
NB = 14
TOPK = 256
BIG = 1.0e30


class Res:
    __slots__ = ("w", "r")

    def __init__(self):
        self.w = {}
        self.r = {}


class Sch:
    def __init__(self, nc, ctx):
        self.nc = nc
        self.eng = {"pe": nc.tensor, "act": nc.scalar, "dve": nc.vector, "pool": nc.gpsimd, "sp": nc.sync}
        self.semobj = {}
        for k in self.eng:
            self.semobj[k] = ctx.enter_context(nc.semaphore("s_" + k))
        self.cnt = {k: 0 for k in self.eng}
        self.seen = {k: {} for k in self.eng}
        self.NR = 16
        self.dcount = {"sp": 0, "pool": 0}
        for q in ("sp", "pool"):
            for i in range(self.NR):
                self.semobj[(q, i)] = ctx.enter_context(nc.semaphore("d_%s%d" % (q, i)))

    def _deps(self, e, reads, writes):
        d = {}

        def add(k, v, same_ok):
            if k == e and e == "pe":
                return
            if d.get(k, 0) < v:
                d[k] = v

        for r in reads:
            for k, v in r.w.items():
                add(k, v, True)
        for w in writes:
            for k, v in w.w.items():
                add(k, v, False)
            for k, v in w.r.items():
                add(k, v, False)
        return d

    def _wait(self, e, deps):
        sn = self.seen[e]
        for k, v in deps.items():
            if sn.get(k, 0) < v:
                self.eng[e].wait_ge(self.semobj[k], v)
                sn[k] = v

    def _mark(self, tag, reads, writes):
        k, v = tag
        for r in reads:
            if r.r.get(k, 0) < v:
                r.r[k] = v
        for w in writes:
            if w.w.get(k, 0) < v:
                w.w[k] = v
            w.r = {}

    def op(self, e, fn, reads=(), writes=(), inc=True):
        self._wait(e, self._deps(e, reads, writes))
        ins = fn(self.eng[e])
        if inc:
            self.cnt[e] += 1
            ins.then_inc(self.semobj[e], 1)
            tag = (e, self.cnt[e])
        else:
            tag = (e, self.cnt[e] + 1)
        self._mark(tag, reads, writes)

    def dma(self, q, out, in_, reads=(), writes=(), slow=False):
        j = self.dcount[q]
        self.dcount[q] += 1
        k = (q, j % self.NR)
        val = 16 * (j // self.NR + 1)
        deps = self._deps(q, reads, writes)
        if val > 16:
            deps[k] = max(deps.get(k, 0), val - 16)
        self._wait(q, deps)
        if slow:
            self.eng[q].dma_start(out=out, in_=in_, allow_slow_non_contiguous=True).then_inc(self.semobj[k], 16)
        else:
            self.eng[q].dma_start(out=out, in_=in_).then_inc(self.semobj[k], 16)
        self._mark((k, val), reads, writes)

    def barrier(self):
        tot = {}
        for k in self.eng:
            if self.cnt[k] > 0:
                tot[k] = self.cnt[k]
        for q in ("sp", "pool"):
            n = self.dcount[q]
            for i in range(self.NR):
                uses = (n - i + self.NR - 1) // self.NR if n > i else 0
                if uses > 0:
                    tot[(q, i)] = 16 * uses
        for e in self.eng:
            self._wait(e, {k: v for k, v in tot.items() if k != e})


def build(S=4096, stop=None, debug=False):
    NT = S // 128
    NS = S // 512
    NQ = S // 256
    nc = bass.Bass("TRN2", target_bir_lowering=False)

    def din(name, shape, dt=F32):
        return nc.dram_tensor(name, list(shape), dt, kind="ExternalInput").ap()

    x = din("x", [S, D])
    g_attn_a = din("g_attn_a", [2, D])
    w_in_a = din("w_in_a", [2, D, A_IN])
    w_o_a = din("w_o_a", [2, D, D])
    g_kv = din("g_kv", [D])
    w_kv_b = din("w_kv_b", [D, KV_IN])
    b_f = din("b_f", [16])
    g_attn_b = din("g_attn_b", [2, D])
    w_q_b = din("w_q_b", [2, D, D])
    w_o_b = din("w_o_b", [2, D, D])
    g_mlp = din("g_mlp", [4, D])
    w_up = din("w_up", [4, D, DFF])
    w_down = din("w_down", [4, DFF, D])
    g_final = din("g_final", [D])
    c_ident = din("c_ident", [128, 128])
    c_ropeC = din("c_ropeC", [128, S])
    c_ropeS = din("c_ropeS", [128, S])
    c_mbA = din("c_mbA", [128, 2, 256])
    c_cbB = din("c_cbB", [128, 4, 512])
    c_pw = din("c_pw", [128, 2 * (NB + 1)])
    out = nc.dram_tensor("out", [S, D], F32, kind="ExternalOutput").ap()

    dk = dict(kind="ExternalOutput") if debug else {}
    hT = nc.dram_tensor("hT", [D, S], F32, **dk).ap()
    qA = nc.dram_tensor("qA", [D, S], BF16, **dk).ap()
    qiA = nc.dram_tensor("qiA", [512, S], BF16, **dk).ap()
    dbg = nc.dram_tensor("dbg", [128, 8192], F32, **dk).ap()
    kaug = nc.dram_tensor("kaug", [16, 70, S], BF16).ap()
    qaug = nc.dram_tensor("qaug", [16, 70, S], BF16).ap()
    vB = nc.dram_tensor("vB", [S, 16, 65], BF16).ap()
    oTs = nc.dram_tensor("oTs", [16, 64, S], BF16).ap()
    hT_v = hT.rearrange("(c p) t -> p c t", p=128)

    with contextlib.ExitStack() as gctx:
        sc = Sch(nc, gctx)

        uid = [0]

        def sb(ctx, name, shape, dt):
            uid[0] += 1
            return ctx.enter_context(nc.sbuf_tensor("%s_u%d" % (name, uid[0]), list(shape), dt))

        P = [gctx.enter_context(nc.psum_tensor("P%d" % i, [128, 512], F32)) for i in range(7)]
        PB = gctx.enter_context(nc.psum_tensor("PB", [128, 1024], BF16))
        RP = [Res() for _ in range(7)]
        RPB = Res()

        ident_f = sb(gctx, "ident_f", [128, 128], F32)
        ident_b = sb(gctx, "ident_b", [128, 128], BF16)
        ones_b = sb(gctx, "ones_b", [128, 128], BF16)
        ones_f = sb(gctx, "ones_f", [128, 64], F32)
        eps_t = sb(gctx, "eps_t", [128, 1], F32)
        one_t = sb(gctx, "one_t", [128, 1], F32)
        gall = sb(gctx, "gall", [128, 10, 8], F32)
        Rc = Res()
        Rg = Res()
        sc.dma("sp", ident_f[:, :], c_ident, writes=[Rc])
        sc.op("dve", lambda e: e.tensor_copy(out=ident_b[:, :], in_=ident_f[:, :]), reads=[Rc], writes=[Rc])
        sc.op("dve", lambda e: e.memset(ones_b[:, :], 1.0), writes=[Rc])
        sc.op("dve", lambda e: e.memset(ones_f[:, :], 1.0), writes=[Rc])
        sc.op("dve", lambda e: e.memset(eps_t[:, :], EPS), writes=[Rc])
        sc.op("dve", lambda e: e.memset(one_t[:, :], 1.0), writes=[Rc])
        glist = [g_attn_a[0], g_attn_a[1], g_kv, g_attn_b[0], g_attn_b[1],
                 g_mlp[0], g_mlp[1], g_mlp[2], g_mlp[3], g_final]
        for gi, gap in enumerate(glist):
            sc.dma("sp", gall[:, gi, :], gap.rearrange("(c p) -> p c", p=128), writes=[Rg], slow=True)
        G_A, G_KV, G_B, G_M, G_F = 0, 2, 3, 5, 9
        RhT = Res()

        def load_w(q_rows_ap, dst, kparts, res):
            v = q_rows_ap.rearrange("(k p) n -> p k n", p=128)
            for k in range(kparts):
                sc.dma("pool", dst[:, k, :], v[:, k, :], writes=[res])

        def norm_tile(ctx_bufs, ht, Rht, gi, xn, Rxn, TW, final=False):
            sq, Rsq, rstd, Rrstd, sstd, Rsstd, bank = ctx_bufs
            sc.op("act", lambda e: e.activation(out=sq[:, :, 0:TW], in_=ht[:, :, 0:TW], func=AF.Square),
                  reads=[Rht], writes=[Rsq])
            for c in range(8):
                sc.op("pe", lambda e, c=c: e.matmul(P[bank][:, 0:TW], lhsT=ones_b[:, :], rhs=sq[:, c, 0:TW],
                                                   start=(c == 0), stop=(c == 7)),
                      reads=[Rsq, Rc], writes=[RP[bank]], inc=(c == 7))
            sc.op("act", lambda e: e.activation(out=sstd[:, 0:TW], in_=P[bank][:, 0:TW], func=AF.Sqrt,
                                                bias=eps_t[:, :], scale=1.0 / D),
                  reads=[RP[bank], Rc], writes=[Rsstd])
            sc.op("dve", lambda e: e.reciprocal(out=rstd[:, 0:TW], in_=sstd[:, 0:TW]),
                  reads=[Rsstd], writes=[Rrstd])
            for c in range(8):
                sc.op("dve", lambda e, c=c: e.scalar_tensor_tensor(
                    out=xn[:, c, 0:TW], in0=ht[:, c, 0:TW], scalar=gall[:, gi, c:c + 1], in1=rstd[:, 0:TW],
                    op0=ALU.mult, op1=ALU.mult), reads=[Rht, Rrstd, Rg], writes=[Rxn])

        def norm_bufs(ctx, TW, bank):
            sq = sb(ctx, "n_sq", [128, 8, TW], BF16)
            rstd = sb(ctx, "n_rstd", [128, TW], F32)
            sstd = sb(ctx, "n_sstd", [128, TW], F32)
            return (sq, Res(), rstd, Res(), sstd, Res(), bank)

        def normalize_heads(Ops, ROps, W_, Osb, ROsb, rden, Rrden, dst_ap, Rdst, bcbank, use_act=False):
            if rden is None:
                rden, Rrden = Osb, ROsb
            sc.op("act", lambda e: e.copy(out=Osb[0:65, 0:W_], in_=Ops[0:65, 0:W_]), reads=[ROps], writes=[ROsb])
            if use_act:
                sc.op("act", lambda e: e.activation(out=rden[64:65, 0:W_], in_=Osb[64:65, 0:W_], func=AF.Ln),
                      reads=[ROsb], writes=[Rrden])
                sc.op("act", lambda e: e.activation(out=rden[64:65, 0:W_], in_=rden[64:65, 0:W_], func=AF.Exp,
                                                    scale=-1.0), reads=[Rrden], writes=[Rrden])
            else:
                sc.op("dve", lambda e: e.reciprocal(out=rden[64:65, 0:W_], in_=Osb[64:65, 0:W_]),
                      reads=[ROsb], writes=[Rrden])
            sc.op("pe", lambda e: e.matmul(P[bcbank][0:64, 0:W_], lhsT=ones_f[64:65, 0:64], rhs=rden[64:65, 0:W_],
                                           start=True, stop=True), reads=[Rrden, Rc], writes=[RP[bcbank]])
            sc.op("dve", lambda e: e.tensor_tensor(out=dst_ap, in0=Osb[0:64, 0:W_], in1=P[bcbank][0:64, 0:W_],
                                                   op=ALU.mult), reads=[ROsb, RP[bcbank]], writes=[Rdst])

        def oproj_tile(Wo, RWo, oT, RoT, ht, Rht, TW, banks):
            for dc in range(8):
                bk = banks[dc % len(banks)]
                for h in range(16):
                    sc.op("pe", lambda e, h=h, dc=dc, bk=bk: e.matmul(
                        P[bk][:, 0:TW], lhsT=Wo[0:64, h, dc * 128:(dc + 1) * 128], rhs=oT[0:64, h, 0:TW],
                        start=(h == 0), stop=(h == 15)), reads=[RWo, RoT], writes=[RP[bk]], inc=(h == 15))
                sc.op("dve", lambda e, dc=dc, bk=bk: e.tensor_tensor(
                    out=ht[:, dc, 0:TW], in0=ht[:, dc, 0:TW], in1=P[bk][:, 0:TW], op=ALU.add),
                    reads=[RP[bk], Rht], writes=[Rht])

        def load_wo(w_o_l, Wo, RWo):
            v = w_o_l.rearrange("(h d) m -> d h m", d=64)
            for h in range(16):
                sc.dma("pool", Wo[0:64, h, :], v[:, h, :], writes=[RWo])

        def phase0():
            with contextlib.ExitStack() as ctx:
                xt = [sb(ctx, "p0_xt%d" % i, [128, 4, D], F32) for i in range(2)]
                ht = [sb(ctx, "p0_ht%d" % i, [128, 8, 512], F32) for i in range(2)]
                Rxt = [Res(), Res()]
                Rht = [Res(), Res()]
                k = 0
                for I in range(NS):
                    b = I % 2
                    sc.dma("sp", xt[b][:, :, :], x[I * 512:(I + 1) * 512, :].rearrange("(j p) d -> p j d", p=128),
                           writes=[Rxt[b]])
                    for c in range(8):
                        bk = k % 4
                        k += 1
                        for j in range(4):
                            sc.op("pe", lambda e, j=j, c=c, bk=bk, b=b: e.transpose(
                                out=P[bk][:, j * 128:(j + 1) * 128], in_=xt[b][:, j, c * 128:(c + 1) * 128],
                                identity=ident_f[:, :]), reads=[Rxt[b], Rc], writes=[RP[bk]], inc=(j == 3))
                        eng = "act" if c % 2 == 0 else "dve"
                        if eng == "act":
                            sc.op("act", lambda e, c=c, bk=bk, b=b: e.copy(out=ht[b][:, c, :], in_=P[bk][:, :]),
                                  reads=[RP[bk]], writes=[Rht[b]])
                        else:
                            sc.op("dve", lambda e, c=c, bk=bk, b=b: e.tensor_copy(out=ht[b][:, c, :], in_=P[bk][:, :]),
                                  reads=[RP[bk]], writes=[Rht[b]])
                    sc.dma("sp", hT_v[:, :, I * 512:(I + 1) * 512], ht[b][:, :, :], reads=[Rht[b]], writes=[RhT])
                sc.barrier()

        def layer_A(l):
            with contextlib.ExitStack() as actx:
                kT = sb(actx, "a_kT", [128, 2, S], BF16)
                vaug = sb(actx, "a_vaug", [128, NT, 4, 65], BF16)
                kiT = sb(actx, "a_kiT", [128, S], BF16)
                wabs = sb(actx, "a_wabs", [128, NT, 8], F32)
                wsgn = sb(actx, "a_wsgn", [128, NT, 8], F32)
                RkT, Rv, Rki, Rw = Res(), Res(), Res(), Res()
                sc.op("pool", lambda e: e.memset(vaug[:, :, :, 64:65], 1.0), writes=[Rv])

                with contextlib.ExitStack() as ctx:
                    Wx = sb(ctx, "a1_Wx", [128, 8, A_IN], BF16)
                    Wsw = sb(ctx, "a1_Wsw", [128, 8, A_IN], BF16)
                    Wki = sb(ctx, "a1_Wki", [128, 8, 128], BF16)
                    Wkisw = sb(ctx, "a1_Wkisw", [128, 8, 128], BF16)
                    RW = Res()
                    wv_ = w_in_a[l].rearrange("(k p) n -> p k n", p=128)
                    for kc in range(8):
                        for (a_, b__, jn) in ((0, 1024, 8), (1024, 1280, 2), (1536, 2048, 4)):
                            for tw in range(2):
                                sc.dma("pool", Wx[:, kc, a_:b__].rearrange("p (j two d) -> p two j d", two=2, d=64)[:, tw],
                                       wv_[:, kc, a_:b__].rearrange("p (two j d) -> p two j d", two=2, d=64)[:, tw],
                                       writes=[RW])
                        sc.dma("pool", Wx[:, kc, 1280:1536], wv_[:, kc, 1280:1536], writes=[RW])
                        sc.dma("pool", Wx[:, kc, 2048:2120], wv_[:, kc, 2048:2120], writes=[RW])
                    kiv = w_in_a[l][:, 2048:2112].rearrange("(k p) n -> p k n", p=128)
                    sc.dma("pool", Wki[:, :, 0:64], kiv, writes=[RW])
                    sc.dma("pool", Wki[:, :, 64:128], kiv, writes=[RW])
                    for (a, b_) in ((0, 1280), (1536, 2112)):
                        src = Wx[:, :, a:b_].rearrange("p k (h two i) -> p k h two i", two=2, i=32)
                        dst = Wsw[:, :, a:b_].rearrange("p k (h two i) -> p k h two i", two=2, i=32)
                        for t in range(2):
                            sc.op("dve", lambda e, src=src, dst=dst, t=t: e.tensor_copy(
                                out=dst[:, :, :, t, :], in_=src[:, :, :, 1 - t, :]), reads=[RW], writes=[RW])
                    srck = Wki[:, :, :].rearrange("p k (h two i) -> p k h two i", two=2, i=32)
                    dstk = Wkisw[:, :, :].rearrange("p k (h two i) -> p k h two i", two=2, i=32)
                    for t in range(2):
                        sc.op("dve", lambda e, t=t: e.tensor_copy(out=dstk[:, :, :, t, :], in_=srck[:, :, :, 1 - t, :]),
                              reads=[RW], writes=[RW])

                    ht = [sb(ctx, "a1_ht%d" % i, [128, 8, 512], F32) for i in range(2)]
                    xn = sb(ctx, "a1_xn", [128, 8, 512], BF16)
                    Ct = [sb(ctx, "a1_C%d" % i, [128, 512], F32) for i in range(2)]
                    St = [sb(ctx, "a1_S%d" % i, [128, 512], F32) for i in range(2)]
                    qt = [sb(ctx, "a1_qt%d" % i, [128, 8, 512], BF16) for i in range(2)]
                    qit = [sb(ctx, "a1_qit%d" % i, [128, 4, 512], BF16) for i in range(2)]
                    t1 = [sb(ctx, "a1_t1%d" % i, [128, 512], F32) for i in range(2)]
                    t2 = [sb(ctx, "a1_t2%d" % i, [128, 512], F32) for i in range(2)]
                    Rht = [Res(), Res()]
                    Rxn = Res()
                    Rtab = [Res(), Res()]
                    Rqt = [Res(), Res()]
                    Rqit = [Res(), Res()]
                    Rt1 = [Res(), Res()]
                    Rt2 = [Res(), Res()]
                    nb = norm_bufs(ctx, 512, 4)

                    def load_tile(I):
                        b = I % 2
                        sc.dma("sp", ht[b][:, :, :], hT_v[:, :, I * 512:(I + 1) * 512], reads=[RhT], writes=[Rht[b]])
                        sc.dma("sp", Ct[b][:, :], c_ropeC[:, I * 512:(I + 1) * 512], writes=[Rtab[b]])
                        sc.dma("sp", St[b][:, :], c_ropeS[:, I * 512:(I + 1) * 512], writes=[Rtab[b]])

                    qv = lambda W, kc: W[:, kc, 0:1024].rearrange("p (j c) -> p j c", c=128)
                    kv_ = lambda W, kc: W[:, kc, 1024:1280].rearrange("p (j c) -> p j c", c=128)
                    qiv = lambda W, kc: W[:, kc, 1536:2048].rearrange("p (j c) -> p j c", c=128)

                    load_tile(0)
                    cn = 0
                    for I in range(NS):
                        b = I % 2
                        if I + 1 < NS:
                            load_tile(I + 1)
                        norm_tile(nb, ht[b], Rht[b], G_A + l, xn, Rxn, 512)
                        chunks = []
                        for j in range(8):
                            chunks.append((lambda kc, j=j: qv(Wx, kc)[:, j], lambda kc, j=j: qv(Wsw, kc)[:, j],
                                           qt[b][:, j, :], Rqt[b]))
                        for j in range(2):
                            chunks.append((lambda kc, j=j: kv_(Wx, kc)[:, j], lambda kc, j=j: kv_(Wsw, kc)[:, j],
                                           kT[:, j, I * 512:(I + 1) * 512], RkT))
                        for j in range(4):
                            chunks.append((lambda kc, j=j: qiv(Wx, kc)[:, j], lambda kc, j=j: qiv(Wsw, kc)[:, j],
                                           qit[b][:, j, :], Rqit[b]))
                        chunks.append((lambda kc: Wki[:, kc, :], lambda kc: Wkisw[:, kc, :],
                                       kiT[:, I * 512:(I + 1) * 512], Rki))
                        for (fa, fb, dst, Rd) in chunks:
                            pb = cn % 2
                            cn += 1
                            pa_, pb_ = 2 * pb, 2 * pb + 1
                            for kc in range(8):
                                sc.op("pe", lambda e, kc=kc, fa=fa, pa_=pa_: e.matmul(
                                    P[pa_][:, :], lhsT=fa(kc), rhs=xn[:, kc, :], start=(kc == 0), stop=(kc == 7)),
                                    reads=[RW, Rxn], writes=[RP[pa_]], inc=(kc == 7))
                            for kc in range(8):
                                sc.op("pe", lambda e, kc=kc, fb=fb, pb_=pb_: e.matmul(
                                    P[pb_][:, :], lhsT=fb(kc), rhs=xn[:, kc, :], start=(kc == 0), stop=(kc == 7)),
                                    reads=[RW, Rxn], writes=[RP[pb_]], inc=(kc == 7))
                            sc.op("dve", lambda e, pa_=pa_, pb=pb, b=b: e.tensor_tensor(
                                out=t1[pb][:, :], in0=P[pa_][:, :], in1=Ct[b][:, :], op=ALU.mult),
                                reads=[RP[pa_], Rtab[b]], writes=[Rt1[pb]])
                            sc.op("dve", lambda e, pb_=pb_, pb=pb, b=b: e.tensor_tensor(
                                out=t2[pb][:, :], in0=P[pb_][:, :], in1=St[b][:, :], op=ALU.mult),
                                reads=[RP[pb_], Rtab[b]], writes=[Rt2[pb]])
                            sc.op("dve", lambda e, dst=dst, pb=pb: e.tensor_tensor(
                                out=dst, in0=t1[pb][:, :], in1=t2[pb][:, :], op=ALU.add),
                                reads=[Rt1[pb], Rt2[pb]], writes=[Rd])
                        for jj in range(4):
                            tile = I * 4 + jj
                            for kc in range(8):
                                sc.op("pe", lambda e, kc=kc, jj=jj: e.matmul(
                                    P[5][:, 0:256], lhsT=xn[:, kc, jj * 128:(jj + 1) * 128], rhs=Wx[:, kc, 1280:1536],
                                    start=(kc == 0), stop=(kc == 7)), reads=[RW, Rxn], writes=[RP[5]], inc=(kc == 7))
                            sc.op("act", lambda e, tile=tile: e.copy(
                                out=vaug[:, tile, :, 0:64], in_=P[5][:, 0:256].rearrange("p (g d) -> p g d", g=4)),
                                reads=[RP[5]], writes=[Rv])
                            for kc in range(8):
                                sc.op("pe", lambda e, kc=kc, jj=jj: e.matmul(
                                    P[6][:, 0:8], lhsT=xn[:, kc, jj * 128:(jj + 1) * 128], rhs=Wx[:, kc, 2112:2120],
                                    start=(kc == 0), stop=(kc == 7)), reads=[RW, Rxn], writes=[RP[6]], inc=(kc == 7))
                            sc.op("act", lambda e, tile=tile: e.activation(out=wabs[:, tile, :], in_=P[6][:, 0:8],
                                                                           func=AF.Abs), reads=[RP[6]], writes=[Rw])
                            sc.op("act", lambda e, tile=tile: e.activation(out=wsgn[:, tile, :], in_=P[6][:, 0:8],
                                                                           func=AF.Sign), reads=[RP[6]], writes=[Rw])
                        RqA = Res()
                        sc.dma("sp", qA.rearrange("(c p) t -> p c t", p=128)[:, :, I * 512:(I + 1) * 512],
                               qt[b][:, :, :], reads=[Rqt[b]], writes=[RqA])
                        sc.dma("sp", qiA.rearrange("(c p) t -> p c t", p=128)[:, :, I * 512:(I + 1) * 512],
                               qit[b][:, :, :], reads=[Rqit[b]], writes=[RqA])
                    sc.barrier()

                with contextlib.ExitStack() as ctx:
                    Wo = sb(ctx, "a2_Wo", [64, 16, D], BF16)
                    RWo = Res()
                    load_wo(w_o_a[l], Wo, RWo)
                    mbA = sb(ctx, "a2_mbA", [128, 2, 256], F32)
                    pw = sb(ctx, "a2_pw", [128, 2 * (NB + 1)], F32)
                    Rk = Res()
                    sc.dma("sp", mbA[:, :, :], c_mbA, writes=[Rk])
                    sc.dma("sp", pw[:, :], c_pw, writes=[Rk])
                    qt = [sb(ctx, "a2_qt%d" % i, [128, 16, 256], BF16) for i in range(2)]
                    qit = [sb(ctx, "a2_qit%d" % i, [128, 4, 256], BF16) for i in range(2)]
                    ht1 = sb(ctx, "a2_ht", [128, 8, 256], F32)
                    ht = [ht1, ht1]
                    score = [sb(ctx, "a2_score%d" % i, [128, S], F32) for i in range(2)]
                    rl = [sb(ctx, "a2_rl%d" % i, [128, 512], F32) for i in range(2)]
                    maskq = [sb(ctx, "a2_mq%d" % i, [128, S], BF16) for i in range(2)]
                    maskT = [sb(ctx, "a2_mT%d" % i, [128, NT, 256], mybir.dt.uint8) for i in range(2)]
                    PT = [sb(ctx, "a2_PT%d" % i, [128, 512], BF16) for i in range(5)]
                    Osb = [sb(ctx, "a2_Osb%d" % i, [128, 512], F32) for i in range(2)]
                    oT = sb(ctx, "a2_oT", [64, 16, 256], BF16)
                    st = [sb(ctx, "a2_st%d" % i, [128, 16], F32) for i in range(2)]
                    wtab = [sb(ctx, "a2_wtab%d" % i, [128, 2 * (NB + 1)], F32) for i in range(2)]
                    midt = [sb(ctx, "a2_midt%d" % i, [128, NB + 2], F32) for i in range(2)]
                    gt = [sb(ctx, "a2_gt%d" % i, [128, NB + 1], F32) for i in range(2)]
                    cnt_t = [sb(ctx, "a2_cnt%d" % i, [128, NB + 1], F32) for i in range(2)]
                    Rqt = [Res(), Res()]
                    Rht1 = Res()
                    Rht = [Rht1, Rht1]
                    Rscore, Rjunk = [Res(), Res()], Res()
                    RS = [Res() for _ in range(4)]
                    Rrl = [Res(), Res()]
                    Rmq = [Res(), Res()]
                    RmT = [Res(), Res()]
                    RPT = [Res() for _ in range(6)]
                    ROsb = [Res(), Res()]
                    Rrden = [Res(), Res()]
                    RoT = Res()
                    Rst = [Res(), Res()]

                    def load_q(T):
                        b = T % 2
                        qAv = qA.rearrange("(c p) t -> p c t", p=128)
                        sc.dma("sp", qt[b][0:64, 0:8, :], qAv[0:64, :, T * 256:(T + 1) * 256], writes=[Rqt[b]])
                        sc.dma("sp", qt[b][64:128, 8:16, :], qAv[64:128, :, T * 256:(T + 1) * 256], writes=[Rqt[b]])
                        sc.dma("sp", qit[b][:, :, :], qiA.rearrange("(c p) t -> p c t", p=128)[:, :, T * 256:(T + 1) * 256],
                               writes=[Rqt[b]])

                    def load_ht(T):
                        sc.dma("sp", ht1[:, :, :], hT_v[:, :, T * 256:(T + 1) * 256], reads=[RhT], writes=[Rht1])

                    for i in range(2):
                        sc.op("pool", lambda e, i=i: e.memset(qt[i][64:128, 0:8, :], 0.0), writes=[Rqt[i]])
                        sc.op("pool", lambda e, i=i: e.memset(qt[i][0:64, 8:16, :], 0.0), writes=[Rqt[i]])
                    load_q(0)
                    ibc = [0]
                    horder = (0, 8, 1, 9, 2, 10, 3, 11, 4, 12, 5, 13, 6, 14, 7, 15)
                    LA = 2
                    NPT = len(PT)
                    mmc = [0]

                    def gen_topk(j, T, b, L):
                        tile = 2 * T + j
                        sco = score[j]
                        Rs = Rscore[j]
                        Rt = Rst[j]
                        st_, wt_, mt_, gt_, ct_ = st[j], wtab[j], midt[j], gt[j], cnt_t[j]
                        for k0 in range(0, L, 512):
                            kw = min(512, L - k0)
                            for hi, h in enumerate((0, 4, 1, 5, 2, 6, 3, 7) if j == 0 else (4, 0, 5, 1, 6, 2, 7, 3)):
                                hb, hc = h // 4, h % 4
                                bk = ibc[0] % 2
                                ibc[0] += 1
                                sc.op("pe", lambda e, hb=hb, hc=hc, bk=bk, k0=k0, kw=kw: e.matmul(
                                    P[bk][:, 0:kw], lhsT=qit[b][hb * 64:(hb + 1) * 64, hc, j * 128:(j + 1) * 128],
                                    rhs=kiT[hb * 64:(hb + 1) * 64, k0:k0 + kw], start=True, stop=True),
                                    reads=[Rqt[b], Rki], writes=[RP[bk]])
                                sc.op("act", lambda e, bk=bk, kw=kw, h=h: e.activation(
                                    out=rl[bk][:, 0:kw], in_=P[bk][:, 0:kw], func=AF.Relu,
                                    scale=wabs[:, tile, h:h + 1]), reads=[RP[bk], Rw], writes=[Rrl[bk]])
                                if hi == 0:
                                    sc.op("dve", lambda e, bk=bk, kw=kw, k0=k0, h=h: e.tensor_scalar(
                                        out=sco[:, k0:k0 + kw], in0=rl[bk][:, 0:kw], scalar1=wsgn[:, tile, h:h + 1],
                                        scalar2=None, op0=ALU.mult), reads=[Rrl[bk], Rw], writes=[Rs])
                                else:
                                    sc.op("dve", lambda e, bk=bk, kw=kw, k0=k0, h=h: e.scalar_tensor_tensor(
                                        out=sco[:, k0:k0 + kw], in0=rl[bk][:, 0:kw], scalar=wsgn[:, tile, h:h + 1],
                                        in1=sco[:, k0:k0 + kw], op0=ALU.mult, op1=ALU.add),
                                        reads=[Rrl[bk], Rw, Rs], writes=[Rs])
                                yield
                        sc.op("dve", lambda e: e.tensor_reduce(out=st_[:, 0:1], in_=sco[:, 0:L], axis=AX.X, op=ALU.min),
                              reads=[Rs], writes=[Rt])
                        sc.op("dve", lambda e: e.tensor_reduce(out=st_[:, 1:2], in_=sco[:, 0:L], axis=AX.X, op=ALU.max),
                              reads=[Rs], writes=[Rt])
                        yield
                        sc.op("dve", lambda e: e.tensor_tensor(out=sco[:, L - 256:L], in0=sco[:, L - 256:L],
                                                               in1=mbA[:, j, :], op=ALU.add),
                              reads=[Rs, Rk, Rt], writes=[Rs])
                        sc.op("dve", lambda e: e.tensor_tensor(out=st_[:, 2:3], in0=st_[:, 1:2], in1=st_[:, 0:1],
                                                               op=ALU.subtract), reads=[Rt], writes=[Rt])
                        yield
                        sc.op("dve", lambda e: e.tensor_scalar(out=wt_[:, :], in0=pw[:, :], scalar1=st_[:, 2:3],
                                                               scalar2=None, op0=ALU.mult), reads=[Rt, Rk], writes=[Rt])
                        yield
                        sc.op("dve", lambda e: e.tensor_tensor(out=mt_[:, 0:1], in0=st_[:, 0:1], in1=wt_[:, 0:1],
                                                               op=ALU.add), reads=[Rt], writes=[Rt])
                        yield
                        for n in range(NB):
                            if j == 0:
                                sc.op("dve", lambda e, n=n: e.tensor_scalar(
                                    out=maskq[j][:, 0:L], in0=sco[:, 0:L], scalar1=mt_[:, n:n + 1], scalar2=None,
                                    op0=ALU.is_ge, op1=ALU.add, accum_out=ct_[:, n:n + 1]),
                                    reads=[Rs, Rt], writes=[Rmq[j], Rt])
                                yield
                                sc.op("dve", lambda e, n=n: e.tensor_scalar(
                                    out=gt_[:, n:n + 1], in0=ct_[:, n:n + 1], scalar1=float(TOPK) - 0.5,
                                    scalar2=wt_[:, NB + 1 + n + 1:NB + 1 + n + 2], op0=ALU.is_ge, op1=ALU.mult),
                                    reads=[Rt], writes=[Rt])
                            else:
                                sc.op("act", lambda e, n=n: e.activation(
                                    out=maskq[j][:, 0:L], in_=sco[:, 0:L], func=AF.Sign, bias=mt_[:, n:n + 1], scale=-1.0,
                                    accum_out=ct_[:, n:n + 1]), reads=[Rs, Rt], writes=[Rmq[j], Rt])
                                yield
                                sc.op("dve", lambda e, n=n: e.tensor_scalar(
                                    out=gt_[:, n:n + 1], in0=ct_[:, n:n + 1], scalar1=float(L - 2 * TOPK + 1),
                                    scalar2=wt_[:, NB + 1 + n + 1:NB + 1 + n + 2], op0=ALU.is_le, op1=ALU.mult),
                                    reads=[Rt], writes=[Rt])
                            yield
                            sc.op("dve", lambda e, n=n: e.scalar_tensor_tensor(
                                out=mt_[:, n + 1:n + 2], in0=mt_[:, n:n + 1], scalar=wt_[:, n + 1:n + 2],
                                in1=gt_[:, n:n + 1], op0=ALU.subtract, op1=ALU.add), reads=[Rt], writes=[Rt])
                            yield
                        sc.op("dve", lambda e: e.tensor_tensor(out=mt_[:, NB + 1:NB + 2], in0=mt_[:, NB:NB + 1],
                                                               in1=wt_[:, NB:NB + 1], op=ALU.subtract),
                              reads=[Rt], writes=[Rt])
                        yield
                        sc.op("dve", lambda e: e.tensor_scalar(
                            out=maskq[j][:, 0:L], in0=sco[:, 0:L], scalar1=mt_[:, NB + 1:NB + 2], scalar2=None,
                            op0=ALU.is_ge), reads=[Rs, Rt], writes=[Rmq[j]])
                        yield


                    def gen_X(T):
                        b = T % 2
                        L = 256 * (T + 1)
                        nch = L // 128
                        alive = [gen_topk(0, T, b, L), gen_topk(1, T, b, L)]
                        while alive:
                            for g_ in list(alive):
                                try:
                                    next(g_)
                                    yield
                                except StopIteration:
                                    alive.remove(g_)
                        mT = maskT[b]
                        for c0 in range(0, nch, 4):
                            cw = min(4, nch - c0)
                            for cc in range(cw):
                                for j in range(2):
                                    last = (cc == cw - 1 and j == 1)
                                    sc.op("pe", lambda e, cc=cc, j=j, c0=c0: e.transpose(
                                        out=PB[:, cc * 256 + j * 128:cc * 256 + (j + 1) * 128],
                                        in_=maskq[j][:, (c0 + cc) * 128:(c0 + cc + 1) * 128], identity=ident_b[:, :]),
                                        reads=[Rmq[j], Rc], writes=[RPB], inc=last)
                            sc.op("act", lambda e, c0=c0, cw=cw: e.copy(
                                out=mT[:, c0:c0 + cw, :], in_=PB[:, 0:cw * 256].rearrange("p (c t) -> p c t", t=256)),
                                reads=[RPB], writes=[RmT[b]])
                            yield

                    def gen_Y(T):
                        b = T % 2
                        L = 256 * (T + 1)
                        nch = L // 128
                        mT = maskT[b]
                        items = [(g, c, p) for g in (0, 2, 1, 3) for c in range(nch) for p in range(2)]

                        def front(i):
                            g, c, p = items[i]
                            hb = g // 2
                            kj = g % 2
                            c0 = 4 * (g % 2) + 2 * p
                            sl = 2 + i % 2
                            pk = i % NPT
                            sc.op("pe", lambda e: e.matmul(
                                P[sl][:, :], lhsT=kT[:, kj, c * 128:(c + 1) * 128],
                                rhs=qt[b][:, hb * 8 + c0:hb * 8 + c0 + 2, :].rearrange("p c t -> p (c t)"),
                                start=True, stop=True), reads=[RkT, Rqt[b]], writes=[RP[sl]])
                            sc.op("act", lambda e: e.activation(
                                out=PT[pk][:, :], in_=P[sl][:, :], func=AF.Exp, scale=0.125),
                                reads=[RP[sl]], writes=[RPT[pk]])
                            mmc[0] += 1
                            meng = "pool" if (mmc[0] % 3 == 0) else "dve"
                            mbase = mT[:, c, :]
                            mbc = bass.AP(mbase.tensor, mbase.offset, [list(mbase.ap[0]), [0, 2], list(mbase.ap[1])])
                            ptv = PT[pk][:, :].rearrange("p (a t) -> p a t", a=2)
                            sc.op(meng, lambda e: e.tensor_tensor(out=ptv, in0=ptv, in1=mbc, op=ALU.mult),
                                  reads=[RPT[pk], RmT[b]], writes=[RPT[pk]])

                        def back(i):
                            g, c, p = items[i]
                            ob = 4 + p
                            pk = i % NPT
                            sc.op("pe", lambda e: e.matmul(
                                P[ob][0:65, :], lhsT=vaug[:, c, g, :], rhs=PT[pk][:, :],
                                start=(c == 0), stop=(c == nch - 1)), reads=[RPT[pk], Rv], writes=[RP[ob]],
                                inc=(c == nch - 1))
                            if c == nch - 1:
                                h0 = 4 * g + 2 * p
                                normalize_heads(P[ob], RP[ob], 512, Osb[p], ROsb[p], None, None,
                                                oT[0:64, h0:h0 + 2, :].rearrange("p a t -> p (a t)"), RoT, 6,
                                                use_act=True)

                        for i in range(len(items) + LA):
                            if i < len(items):
                                front(i)
                            if i - LA >= 0:
                                back(i - LA)
                            yield
                        oproj_tile(Wo, RWo, oT, RoT, ht[b], Rht[b], 256, (2, 3))
                        sc.dma("sp", hT_v[:, :, T * 256:(T + 1) * 256], ht[b][:, :, :], reads=[Rht[b]], writes=[RhT])
                        yield

                    def nsteps_X(T):
                        L = 256 * (T + 1)
                        return 2 * (((L + 511) // 512) * 8 + 6 + 3 * NB + 2) + (L // 128 + 3) // 4

                    def nsteps_Y(T):
                        return 8 * (256 * (T + 1) // 128) + LA + 1

                    for _ in gen_X(0):
                        pass
                    for T in range(NQ):
                        if T + 1 < NQ:
                            load_q(T + 1)
                        load_ht(T)
                        gy = gen_Y(T)
                        gx = gen_X(T + 1) if T + 1 < NQ else None
                        ratio = (nsteps_X(T + 1) / float(nsteps_Y(T))) if gx is not None else 0.0
                        acc = 0.0
                        for _ in gy:
                            if gx is not None:
                                acc += ratio
                                while acc >= 1.0 and gx is not None:
                                    acc -= 1.0
                                    try:
                                        next(gx)
                                    except StopIteration:
                                        gx = None
                        if gx is not None:
                            for _ in gx:
                                pass
                    sc.barrier()

        def phase_mlp(layer):
            with contextlib.ExitStack() as ctx:
                Wup = sb(ctx, "m_Wup", [128, 8, DFF], BF16)
                Wdn = sb(ctx, "m_Wdn", [128, 32, D], BF16)
                RWu = [Res() for _ in range(4)]
                RWd = [Res() for _ in range(4)]
                wuv = w_up[layer].rearrange("(k p) n -> p k n", p=128)
                wdv = w_down[layer].rearrange("(k p) n -> p k n", p=128)
                for fb in range(4):
                    for k in range(8):
                        sc.dma("pool", Wup[:, k, fb * 1024:(fb + 1) * 1024], wuv[:, k, fb * 1024:(fb + 1) * 1024],
                               writes=[RWu[fb]])
                for fb in range(4):
                    for k in range(8 * fb, 8 * fb + 8):
                        sc.dma("pool", Wdn[:, k, :], wdv[:, k, :], writes=[RWd[fb]])
                TW = 256
                ht = [sb(ctx, "m_ht%d" % i, [128, 8, TW], F32) for i in range(2)]
                xn = [sb(ctx, "m_xn%d" % i, [128, 8, TW], BF16) for i in range(2)]
                rl = [sb(ctx, "m_rl%d" % i, [128, TW], F32) for i in range(4)]
                act = [sb(ctx, "m_act%d" % i, [128, 32, TW], BF16) for i in range(2)]
                Rht = [Res(), Res()]
                Rxn = [Res(), Res()]
                Rrl = [Res() for _ in range(4)]
                Ract = [Res(), Res()]
                nb = norm_bufs(ctx, TW, 6)
                sc.dma("sp", ht[0][:, :, :], hT_v[:, :, 0:TW], reads=[RhT], writes=[Rht[0]])
                uk = 0
                for T in range(S // TW):
                    b = T % 2
                    if T + 1 < S // TW:
                        sc.dma("sp", ht[1 - b][:, :, :], hT_v[:, :, (T + 1) * TW:(T + 2) * TW], reads=[RhT],
                               writes=[Rht[1 - b]])
                    norm_tile(nb, ht[b], Rht[b], G_M + layer, xn[b], Rxn[b], TW)
                    for fc in range(32):
                        bk = uk % 4
                        uk += 1
                        for kc in range(8):
                            sc.op("pe", lambda e, kc=kc, fc=fc, bk=bk, b=b: e.matmul(
                                P[bk][:, 0:TW], lhsT=Wup[:, kc, fc * 128:(fc + 1) * 128], rhs=xn[b][:, kc, :],
                                start=(kc == 0), stop=(kc == 7)), reads=[RWu[fc // 8], Rxn[b]], writes=[RP[bk]], inc=(kc == 7))
                        sc.op("act", lambda e, bk=bk: e.activation(out=rl[bk][:, :], in_=P[bk][:, 0:TW], func=AF.Relu),
                              reads=[RP[bk]], writes=[Rrl[bk]])
                        meng = "dve"
                        sc.op(meng, lambda e, bk=bk, fc=fc, b=b: e.tensor_tensor(
                            out=act[b][:, fc, :], in0=rl[bk][:, :], in1=rl[bk][:, :], op=ALU.mult),
                            reads=[Rrl[bk]], writes=[Ract[b]])
                    for dc in range(8):
                        bk = 4 + dc % 2
                        for fc in range(32):
                            sc.op("pe", lambda e, fc=fc, dc=dc, bk=bk, b=b: e.matmul(
                                P[bk][:, 0:TW], lhsT=Wdn[:, fc, dc * 128:(dc + 1) * 128], rhs=act[b][:, fc, :],
                                start=(fc == 0), stop=(fc == 31)), reads=[RWd[fc // 8], Ract[b]], writes=[RP[bk]], inc=(fc == 31))
                        sc.op("dve", lambda e, dc=dc, bk=bk, b=b: e.tensor_tensor(
                            out=ht[b][:, dc, :], in0=ht[b][:, dc, :], in1=P[bk][:, 0:TW], op=ALU.add),
                            reads=[RP[bk], Rht[b]], writes=[Rht[b]])
                    sc.dma("sp", hT_v[:, :, T * TW:(T + 1) * TW], ht[b][:, :, :], reads=[Rht[b]], writes=[RhT])
                sc.barrier()

        def phase_kv_full():
            octx = contextlib.ExitStack()
            flog = sb(octx, "kv_flog", [16, S], F32)
            Rfl = Res()
            Rkaug, Rqaug, RvB = Res(), Res(), Res()
            with contextlib.ExitStack() as ctx:
                Wkv = sb(ctx, "kv_W", [128, 8, KV_IN], BF16)
                RW = Res()
                load_w(w_kv_b, Wkv, 8, RW)
                ht = [sb(ctx, "kv_ht%d" % i, [128, 8, 512], F32) for i in range(2)]
                xn = sb(ctx, "kv_xn", [128, 8, 512], BF16)
                kst = [sb(ctx, "kv_kst%d" % i, [128, 8, 512], BF16) for i in range(2)]
                vst = [sb(ctx, "kv_vst%d" % i, [128, 4, 16, 65], BF16) for i in range(2)]
                Rht = [Res(), Res()]
                Rxn = Res()
                Rkst = [Res(), Res()]
                Rvst = [Res(), Res()]
                nb = norm_bufs(ctx, 512, 6)
                for i in range(2):
                    sc.op("pool", lambda e, i=i: e.memset(vst[i][:, :, :, 64:65], 1.0), writes=[Rvst[i]])
                sc.dma("sp", ht[0][:, :, :], hT_v[:, :, 0:512], reads=[RhT], writes=[Rht[0]])
                kaug_v = kaug.rearrange("(c b) r t -> b r c t", b=2)
                pk = 0
                for I in range(NS):
                    b = I % 2
                    if I + 1 < NS:
                        sc.dma("sp", ht[1 - b][:, :, :], hT_v[:, :, (I + 1) * 512:(I + 2) * 512], reads=[RhT],
                               writes=[Rht[1 - b]])
                    norm_tile(nb, ht[b], Rht[b], G_KV, xn, Rxn, 512)
                    for c in range(8):
                        bk = pk % 4
                        pk += 1
                        for kc in range(8):
                            sc.op("pe", lambda e, kc=kc, c=c, bk=bk: e.matmul(
                                P[bk][:, :], lhsT=Wkv[:, kc, c * 128:(c + 1) * 128], rhs=xn[:, kc, :],
                                start=(kc == 0), stop=(kc == 7)), reads=[RW, Rxn], writes=[RP[bk]], inc=(kc == 7))
                        sc.op("act", lambda e, c=c, bk=bk, b=b: e.copy(out=kst[b][:, c, :], in_=P[bk][:, :]),
                              reads=[RP[bk]], writes=[Rkst[b]])
                    for hb in range(2):
                        sc.dma("sp", kaug_v[hb, 0:64, :, I * 512:(I + 1) * 512], kst[b][hb * 64:(hb + 1) * 64, :, :],
                               reads=[Rkst[b]], writes=[Rkaug])
                    for jj in range(4):
                        for half in range(2):
                            bk = pk % 4
                            pk += 1
                            for kc in range(8):
                                sc.op("pe", lambda e, kc=kc, jj=jj, half=half, bk=bk: e.matmul(
                                    P[bk][:, :], lhsT=xn[:, kc, jj * 128:(jj + 1) * 128],
                                    rhs=Wkv[:, kc, 1024 + half * 512:1024 + (half + 1) * 512],
                                    start=(kc == 0), stop=(kc == 7)), reads=[RW, Rxn], writes=[RP[bk]], inc=(kc == 7))
                            sc.op("act", lambda e, jj=jj, half=half, bk=bk, b=b: e.copy(
                                out=vst[b][:, jj, half * 8:(half + 1) * 8, 0:64],
                                in_=P[bk][:, :].rearrange("p (h d) -> p h d", d=64)), reads=[RP[bk]], writes=[Rvst[b]])
                    sc.dma("sp", vB[I * 512:(I + 1) * 512].rearrange("(j p) h e -> p j h e", p=128), vst[b][:, :, :, :],
                           reads=[Rvst[b]], writes=[RvB])
                    for kc in range(8):
                        sc.op("pe", lambda e, kc=kc: e.matmul(
                            P[4][0:16, :], lhsT=Wkv[:, kc, 2048:2064], rhs=xn[:, kc, :],
                            start=(kc == 0), stop=(kc == 7)), reads=[RW, Rxn], writes=[RP[4]], inc=(kc == 7))
                    sc.op("act", lambda e, I=I: e.copy(out=flog[0:16, I * 512:(I + 1) * 512], in_=P[4][0:16, :]),
                          reads=[RP[4]], writes=[Rfl])
                sc.barrier()
            with contextlib.ExitStack() as ctx:
                ex = sb(ctx, "kv_ex", [16, S], F32)
                onesS = sb(ctx, "kv_ones", [16, S], F32)
                cum = sb(ctx, "kv_cum", [16, S], F32)
                qrows = sb(ctx, "kv_qrows", [16, 3, S], BF16)
                krows = sb(ctx, "kv_krows", [16, 3, S], BF16)
                orows = sb(ctx, "kv_orows", [16, 3, S], BF16)
                nbf = sb(ctx, "kv_nbf", [16, 1], F32)
                Rm = Res()
                sc.op("pool", lambda e: e.memset(onesS[:, :], 1.0), writes=[Rm])
                sc.op("pool", lambda e: e.memset(orows[:, :, :], 1.0), writes=[Rm])
                sc.dma("sp", nbf[:, :], b_f.rearrange("(p o) -> p o", o=1), writes=[Rm])
                sc.op("dve", lambda e: e.tensor_scalar(out=nbf[:, :], in0=nbf[:, :], scalar1=-1.0, scalar2=None,
                                                       op0=ALU.mult), reads=[Rm], writes=[Rm])
                sc.op("act", lambda e: e.activation(out=ex[:, :], in_=flog[:, :], func=AF.Exp, bias=nbf[:, :], scale=-1.0),
                      reads=[Rfl, Rm], writes=[Rm])
                sc.op("act", lambda e: e.activation(out=ex[:, :], in_=ex[:, :], func=AF.Ln, bias=one_t[0:16, :], scale=1.0),
                      reads=[Rm, Rc], writes=[Rm])
                sc.op("dve", lambda e: e.tensor_tensor_scan(out=cum[:, :], data0=onesS[:, :], data1=ex[:, :], initial=0.0,
                                                            op0=ALU.mult, op1=ALU.subtract), reads=[Rm], writes=[Rm])
                sc.op("dve", lambda e: e.tensor_scalar(out=cum[:, :], in0=cum[:, :], scalar1=8.0, scalar2=None,
                                                       op0=ALU.mult), reads=[Rm], writes=[Rm])
                for r in range(3):
                    sc.op("dve", lambda e, r=r: e.tensor_copy(out=qrows[:, r, :], in_=cum[:, :]), reads=[Rm], writes=[Rm])
                    if r < 2:
                        sc.op("dve", lambda e, r=r: e.tensor_tensor(out=cum[:, :], in0=cum[:, :], in1=qrows[:, r, :],
                                                                    op=ALU.subtract), reads=[Rm], writes=[Rm])
                sc.op("dve", lambda e: e.tensor_scalar(out=krows[:, :, :], in0=qrows[:, :, :], scalar1=-1.0, scalar2=None,
                                                       op0=ALU.mult), reads=[Rm], writes=[Rm])
                sc.dma("sp", qaug[:, 64:67, :], qrows[:, :, :], reads=[Rm], writes=[Rqaug])
                sc.dma("sp", qaug[:, 67:70, :], orows[:, :, :], reads=[Rm], writes=[Rqaug])
                sc.dma("sp", kaug[:, 64:67, :], orows[:, :, :], reads=[Rm], writes=[Rkaug])
                sc.dma("sp", kaug[:, 67:70, :], krows[:, :, :], reads=[Rm], writes=[Rkaug])
                sc.barrier()
            octx.close()

        def layer_B(l):
            with contextlib.ExitStack() as ctx:
                Wq = sb(ctx, "b1_W", [128, 8, D], BF16)
                RW = Res()
                load_w(w_q_b[l], Wq, 8, RW)
                ht = [sb(ctx, "b1_ht%d" % i, [128, 8, 512], F32) for i in range(2)]
                xn = sb(ctx, "b1_xn", [128, 8, 512], BF16)
                qst = [sb(ctx, "b1_qst%d" % i, [128, 8, 512], BF16) for i in range(2)]
                Rht = [Res(), Res()]
                Rxn = Res()
                Rqst = [Res(), Res()]
                Rq = Res()
                nb = norm_bufs(ctx, 512, 6)
                qaug_v = qaug.rearrange("(c b) r t -> b r c t", b=2)
                sc.dma("sp", ht[0][:, :, :], hT_v[:, :, 0:512], reads=[RhT], writes=[Rht[0]])
                pk = 0
                for I in range(NS):
                    b = I % 2
                    if I + 1 < NS:
                        sc.dma("sp", ht[1 - b][:, :, :], hT_v[:, :, (I + 1) * 512:(I + 2) * 512], reads=[RhT],
                               writes=[Rht[1 - b]])
                    norm_tile(nb, ht[b], Rht[b], G_B + l, xn, Rxn, 512)
                    for c in range(8):
                        bk = pk % 4
                        pk += 1
                        for kc in range(8):
                            sc.op("pe", lambda e, kc=kc, c=c, bk=bk: e.matmul(
                                P[bk][:, :], lhsT=Wq[:, kc, c * 128:(c + 1) * 128], rhs=xn[:, kc, :],
                                start=(kc == 0), stop=(kc == 7)), reads=[RW, Rxn], writes=[RP[bk]], inc=(kc == 7))
                        sc.op("act", lambda e, c=c, bk=bk, b=b: e.copy(out=qst[b][:, c, :], in_=P[bk][:, :]),
                              reads=[RP[bk]], writes=[Rqst[b]])
                    for hb in range(2):
                        sc.dma("sp", qaug_v[hb, 0:64, :, I * 512:(I + 1) * 512], qst[b][hb * 64:(hb + 1) * 64, :, :],
                               reads=[Rqst[b]], writes=[Rq])
                sc.barrier()
            with contextlib.ExitStack() as ctx:
                kh = [sb(ctx, "b2_kh%d" % i, [128, S], BF16) for i in range(2)]
                qh = [sb(ctx, "b2_qh%d" % i, [128, S], BF16) for i in range(2)]
                vh = [sb(ctx, "b2_vh%d" % i, [128, NT, 65], BF16) for i in range(2)]
                cb = sb(ctx, "b2_cb", [128, 4, 512], F32)
                PT = [sb(ctx, "b2_PT%d" % i, [128, 512], BF16) for i in range(6)]
                stmp = [sb(ctx, "b2_st%d" % i, [128, 512], F32) for i in range(3)]
                Osb = [sb(ctx, "b2_Osb%d" % i, [128, 512], F32) for i in range(2)]
                rden = [sb(ctx, "b2_rden%d" % i, [128, 512], F32) for i in range(2)]
                oTh = [sb(ctx, "b2_oTh%d" % i, [64, 512], BF16) for i in range(2)]
                Rh = [Res(), Res()]
                Rcb = Res()
                RPT = [Res() for _ in range(6)]
                Rstmp = [Res(), Res(), Res()]
                ROsb = [Res(), Res()]
                Rrden = [Res(), Res()]
                RoTh = [Res(), Res()]
                Ro = Res()
                sc.dma("sp", cb[:, :, :], c_cbB, writes=[Rcb])

                def load_head(h):
                    hb_ = h % 2
                    sc.dma("sp", kh[hb_][0:70, :], kaug[h], writes=[Rh[hb_]])
                    sc.dma("sp", qh[hb_][0:70, :], qaug[h], writes=[Rh[hb_]])
                    sc.dma("sp", vh[hb_][:, :, :], vB[:, h, :].rearrange("(c p) e -> p c e", p=128), writes=[Rh[hb_]])

                load_head(0)
                load_head(1)
                items = []
                ti = 0
                for h in range(16):
                    for T in range(NS):
                        nch = 4 * (T + 1)
                        for c in range(nch):
                            items.append((h, T, c, nch, ti))
                        ti += 1
                LA = 3
                NPT = len(PT)
                dkc = [0]

                def front(i):
                    h, T, c, nch, ti_ = items[i]
                    hb_ = h % 2
                    sk = i % 4
                    pk = i % NPT
                    sc.op("pe", lambda e: e.matmul(
                        P[sk][:, :], lhsT=kh[hb_][0:70, c * 128:(c + 1) * 128],
                        rhs=qh[hb_][0:70, T * 512:(T + 1) * 512], start=True, stop=True),
                        reads=[Rh[hb_]], writes=[RP[sk]])
                    if c >= 4 * T:
                        d_ = dkc[0] % len(stmp)
                        dkc[0] += 1
                        cc = c - 4 * T
                        sc.op("dve", lambda e: e.tensor_tensor(
                            out=stmp[d_][:, :], in0=P[sk][:, :], in1=cb[:, cc, :], op=ALU.add),
                            reads=[RP[sk], Rcb], writes=[Rstmp[d_]])
                        sc.op("act", lambda e: e.activation(
                            out=PT[pk][:, :], in_=stmp[d_][:, :], func=AF.Exp, scale=0.125),
                            reads=[Rstmp[d_]], writes=[RPT[pk]])
                    else:
                        sc.op("act", lambda e: e.activation(
                            out=PT[pk][:, :], in_=P[sk][:, :], func=AF.Exp, scale=0.125),
                            reads=[RP[sk]], writes=[RPT[pk]])

                def back(i):
                    h, T, c, nch, ti_ = items[i]
                    hb_ = h % 2
                    pk = i % NPT
                    ob = 4 + ti_ % 2
                    ob_i = ti_ % 2
                    sc.op("pe", lambda e: e.matmul(
                        P[ob][0:65, :], lhsT=vh[hb_][:, c, :], rhs=PT[pk][:, :],
                        start=(c == 0), stop=(c == nch - 1)), reads=[RPT[pk], Rh[hb_]], writes=[RP[ob]],
                        inc=(c == nch - 1))
                    if c == nch - 1:
                        normalize_heads(P[ob], RP[ob], 512, Osb[ob_i], ROsb[ob_i], rden[ob_i], Rrden[ob_i],
                                        oTh[ob_i][:, :], RoTh[ob_i], 6)
                        sc.dma("sp", oTs[h, :, T * 512:(T + 1) * 512], oTh[ob_i][:, :], reads=[RoTh[ob_i]], writes=[Ro])
                        if T == NS - 1 and h + 2 < 16:
                            load_head(h + 2)

                for i in range(len(items) + LA):
                    if i < len(items):
                        front(i)
                    if i - LA >= 0:
                        back(i - LA)
                sc.barrier()
            with contextlib.ExitStack() as ctx:
                Wo = sb(ctx, "b3_Wo", [64, 16, D], BF16)
                RWo = Res()
                load_wo(w_o_b[l], Wo, RWo)
                ht = [sb(ctx, "b3_ht%d" % i, [128, 8, 512], F32) for i in range(2)]
                oT = [sb(ctx, "b3_oT%d" % i, [64, 16, 512], BF16) for i in range(2)]
                Rht = [Res(), Res()]
                RoT = [Res(), Res()]
                oTs_v = oTs.rearrange("h d t -> d h t")

                def ld(I):
                    b = I % 2
                    sc.dma("sp", ht[b][:, :, :], hT_v[:, :, I * 512:(I + 1) * 512], reads=[RhT], writes=[Rht[b]])
                    sc.dma("sp", oT[b][:, :, :], oTs_v[:, :, I * 512:(I + 1) * 512], writes=[RoT[b]])

                ld(0)
                for I in range(NS):
                    b = I % 2
                    if I + 1 < NS:
                        ld(I + 1)
                    oproj_tile(Wo, RWo, oT[b], RoT[b], ht[b], Rht[b], 512, (0, 1, 2, 3))
                    sc.dma("sp", hT_v[:, :, I * 512:(I + 1) * 512], ht[b][:, :, :], reads=[Rht[b]], writes=[RhT])
                sc.barrier()

        def phase_final(dump_raw=False):
            with contextlib.ExitStack() as ctx:
                ht = [sb(ctx, "f_ht%d" % i, [128, 8, 512], F32) for i in range(2)]
                xn = [sb(ctx, "f_xn%d" % i, [128, 8, 512], F32) for i in range(2)]
                ot = [sb(ctx, "f_ot%d" % i, [128, 4, D], F32) for i in range(2)]
                Rht = [Res(), Res()]
                Rxn = [Res(), Res()]
                Rot = [Res(), Res()]
                Rout = Res()
                nb = norm_bufs(ctx, 512, 6)
                sc.dma("sp", ht[0][:, :, :], hT_v[:, :, 0:512], reads=[RhT], writes=[Rht[0]])
                k = 0
                for I in range(NS):
                    b = I % 2
                    if I + 1 < NS:
                        sc.dma("sp", ht[1 - b][:, :, :], hT_v[:, :, (I + 1) * 512:(I + 2) * 512], reads=[RhT],
                               writes=[Rht[1 - b]])
                    if dump_raw:
                        src, Rsrc = ht[b], Rht[b]
                    else:
                        norm_tile(nb, ht[b], Rht[b], G_F, xn[b], Rxn[b], 512, final=True)
                        src, Rsrc = xn[b], Rxn[b]
                    for j in range(4):
                        for cg in range(2):
                            bk = k % 4
                            k += 1
                            for cc in range(4):
                                c = cg * 4 + cc
                                sc.op("pe", lambda e, j=j, c=c, cc=cc, bk=bk, src=src: e.transpose(
                                    out=P[bk][:, cc * 128:(cc + 1) * 128], in_=src[:, c, j * 128:(j + 1) * 128],
                                    identity=ident_f[:, :]), reads=[Rsrc, Rc], writes=[RP[bk]], inc=(cc == 3))
                            if (j * 2 + cg) % 2 == 0:
                                sc.op("act", lambda e, j=j, cg=cg, bk=bk, b=b: e.copy(
                                    out=ot[b][:, j, cg * 512:(cg + 1) * 512], in_=P[bk][:, :]),
                                    reads=[RP[bk]], writes=[Rot[b]])
                            else:
                                sc.op("dve", lambda e, j=j, cg=cg, bk=bk, b=b: e.tensor_copy(
                                    out=ot[b][:, j, cg * 512:(cg + 1) * 512], in_=P[bk][:, :]),
                                    reads=[RP[bk]], writes=[Rot[b]])
                    sc.dma("sp", out[I * 512:(I + 1) * 512, :].rearrange("(j p) d -> p j d", p=128), ot[b][:, :, :],
                           reads=[Rot[b]], writes=[Rout])
                sc.barrier()

        plan = ["p0", "A0", "M0", "A1", "M1", "KV", "B0", "M2", "B1", "M3"]
        phase0()
        done = False
        for ph in plan[1:]:
            if stop is not None and plan.index(ph) > plan.index(stop):
                done = True
                break
            if ph[0] == "A":
                layer_A(int(ph[1]))
            elif ph[0] == "M":
                phase_mlp(int(ph[1]))
            elif ph == "KV":
                phase_kv_full()
            elif ph[0] == "B":
                layer_B(int(ph[1]))
        phase_final(dump_raw=(stop is not None))
    return nc


def make_consts(S):
    ident = np.eye(128, dtype=np.float32)
    d = 64
    inv = (1.0 / (np.float32(10000.0) ** (np.arange(0, d, 2, dtype=np.float32) / np.float32(d)))).astype(np.float32)
    ang = (np.arange(S, dtype=np.float32)[:, None] * inv[None, :]).astype(np.float32)
    cs = np.cos(ang).astype(np.float32).T
    sn = np.sin(ang).astype(np.float32).T
    p = np.arange(128)
    ropeC = cs[p % 32, :]
    sign = np.where((p % 64) < 32, -1.0, 1.0).astype(np.float32)
    ropeS = sn[p % 32, :] * sign[:, None]
    mbA = np.zeros((128, 2, 256), np.float32)
    r = np.arange(128)[:, None]
    s = np.arange(256)[None, :]
    for j in range(2):
        mbA[:, j, :] = np.where(s >= 128 * j + 64 * (r // 64 + 1), -BIG, 0.0)
    cbB = np.zeros((128, 4, 512), np.float32)
    t = np.arange(512)[None, :]
    for cc in range(4):
        cbB[:, cc, :] = np.where((128 * cc + r) > t, -1.0e9, 0.0)
    pw1 = np.array([2.0 ** -(n + 1) for n in range(NB + 1)], np.float32)
    pw = np.concatenate([pw1, 2 * pw1])[None, :].repeat(128, 0).astype(np.float32)
    return {"c_ident": ident, "c_ropeC": np.ascontiguousarray(ropeC, np.float32),
            "c_ropeS": np.ascontiguousarray(ropeS, np.float32), "c_mbA": mbA, "c_cbB": cbB, "c_pw": pw}


_NC_CACHE = {}


def kernel(**inputs):
    S = inputs["x"].shape[1]
    B = inputs["x"].shape[0]
    if S not in _NC_CACHE:
        _NC_CACHE[S] = build(S)
    nc = _NC_CACHE[S]
    consts = make_consts(S)
    in_maps = []
    for b in range(B):
        m = {k: np.ascontiguousarray(np.asarray(v, dtype=np.float32)) for k, v in inputs.items() if k != "x"}
        m["x"] = np.ascontiguousarray(np.asarray(inputs["x"][b], dtype=np.float32))
        m.update(consts)
        in_maps.append(m)
    res = run_bass_kernel_spmd(nc, in_maps, core_ids=list(range(B)))
    return np.stack([np.asarray(r["out"], dtype=np.float32) for r in res.results], axis=0)
```
